# Optimizing a Trainium2 kernel written in Bass

```python
import math
import jax, jax.numpy as jnp
from jax import lax
import numpy as np

D_MODEL = 2048
BATCH = 2
SEQ = 4096
DEPTH = 2

GRID_W = 64
CTX_LEN = 256
HEAD_DIM = 128
A_Q_HEADS = 6
A_KV_HEADS = 2
B_HEADS = 4
B_QK_DIM = 64
B_V_DIM = 2 * B_QK_DIM
C_Q_HEADS = 6
C_KV_HEADS = 2
WINDOW = 128
QBLK = 128
N_EXPERTS = 16
EXPERT_FF = 1024
EC_CAPACITY = 2
ROPE_THETA = 10000.0
EPS = 1e-6
NEG_INF = -1e30

A_Q_W = A_Q_HEADS * HEAD_DIM
A_KV_W = A_KV_HEADS * HEAD_DIM
B_QK_W = B_HEADS * 2 * B_QK_DIM
B_V_W = B_HEADS * B_V_DIM
C_Q_W = C_Q_HEADS * HEAD_DIM
C_KV_W = C_KV_HEADS * HEAD_DIM
KV_W = 2 * A_KV_W + B_QK_W + B_V_W + 2 * C_KV_W
Q_W = A_Q_W + B_QK_W + C_Q_W
GATE_W = 3 * D_MODEL
IN_W = KV_W + Q_W + GATE_W
KV_SPLITS = (A_KV_W, 2 * A_KV_W, 2 * A_KV_W + B_QK_W, 2 * A_KV_W + B_QK_W + B_V_W,
             2 * A_KV_W + B_QK_W + B_V_W + C_KV_W)
Q_SPLITS = (A_Q_W, A_Q_W + B_QK_W)

kernel_name = "hybrid_parallel_gqa_diff_window_ec_moe"


def rms_norm(x, g):
    xf = x.astype(jnp.float32)
    y = xf * lax.rsqrt(jnp.mean(xf * xf, axis=-1, keepdims=True) + EPS)
    return y.astype(x.dtype) * g


def modulate(h, shift, scale):
    return h * (1 + scale) + shift


def axial_rope_tables(row, col, dh, dtype):
    d_ax = dh // 2
    inv = ROPE_THETA ** (-jnp.arange(0, d_ax, 2, dtype=jnp.float32) / d_ax)
    fr = row.astype(jnp.float32)[:, None] * inv
    fc = col.astype(jnp.float32)[:, None] * inv
    ang = jnp.concatenate([fr, fr, fc, fc], axis=-1)
    return jnp.cos(ang)[:, None, :].astype(dtype), jnp.sin(ang)[:, None, :].astype(dtype)


def rotate_quarters(x):
    x1, x2, x3, x4 = jnp.split(x, 4, axis=-1)
    return jnp.concatenate([-x2, x1, -x4, x3], axis=-1)


def apply_rope(t, rope):
    cos, sin = rope
    return t * cos + rotate_quarters(t) * sin


def qk_heads(t, n, d, gain, rope):
    t = rms_norm(t.reshape(t.shape[0], t.shape[1], n, d), gain)
    if rope is not None:
        t = apply_rope(t, rope)
    return t.transpose(0, 2, 1, 3)


def v_heads(t, n, d):
    return t.reshape(t.shape[0], t.shape[1], n, d).transpose(0, 2, 1, 3)


def group_heads(t, n_kv):
    b, h, tt, d = t.shape
    return t.reshape(b, n_kv, h // n_kv, tt, d)


def split_pairs(t):
    b, h2, tt, d = t.shape
    t = t.reshape(b, h2 // 2, 2, tt, d)
    return t[:, :, 0], t[:, :, 1]


def heads_to_tokens(o):
    b, tt, d = o.shape[0], o.shape[-2], o.shape[-1]
    return o.reshape(b, -1, tt, d).transpose(0, 2, 1, 3).reshape(b, tt, -1)


def gqa_block(q, k, v, sink=None):
    s = jnp.einsum('bhgqd,bhkd->bhgqk', q, k).astype(jnp.float32) * (q.shape[-1] ** -0.5)
    if sink is not None:
        s_sink = jnp.broadcast_to(sink.astype(jnp.float32)[None, :, :, None, None], s.shape[:-1] + (1,))
        p = jax.nn.softmax(jnp.concatenate([s, s_sink], axis=-1), axis=-1)[..., :-1]
    else:
        p = jax.nn.softmax(s, axis=-1)
    return jnp.einsum('bhgqk,bhkd->bhgqd', p.astype(v.dtype), v)


def diff_block(q1, q2, k1, k2, v, lam):
    scale = q1.shape[-1] ** -0.5
    s1 = jnp.einsum('bhqd,bhkd->bhqk', q1, k1).astype(jnp.float32) * scale
    s2 = jnp.einsum('bhqd,bhkd->bhqk', q2, k2).astype(jnp.float32) * scale
    p = jax.nn.softmax(s1, axis=-1) - lam * jax.nn.softmax(s2, axis=-1)
    return jnp.einsum('bhqk,bhkd->bhqd', p.astype(v.dtype), v)


def sweep_blocks(fn, qs):
    nb = qs[0].shape[-2] // QBLK

    def to_blocks(q):
        return jnp.moveaxis(q.reshape(q.shape[:-2] + (nb, QBLK, q.shape[-1])), -3, 0)

    o = lax.map(lambda qb: fn(*qb), tuple(to_blocks(q) for q in qs))
    o = jnp.moveaxis(o, 0, -3)
    return o.reshape(o.shape[:-3] + (nb * QBLK, o.shape[-1]))


def window_attention(q, k, v, k_ctx, v_ctx, sink):
    b, hkv, g, s_len, d = q.shape
    nb = s_len // QBLK
    scale = d ** -0.5
    qb = q.reshape(b, hkv, g, nb, QBLK, d)

    def band(t):
        tp = jnp.pad(t, ((0, 0), (0, 0), (QBLK, QBLK), (0, 0))).reshape(b, hkv, nb + 2, QBLK, d)
        return jnp.concatenate([tp[:, :, :-2], tp[:, :, 1:-1], tp[:, :, 2:]], axis=3)

    kb, vb = band(k), band(v)
    s_loc = jnp.einsum('bhgnqd,bhnkd->bhgnqk', qb, kb).astype(jnp.float32) * scale
    blk = jnp.arange(nb)[:, None, None]
    qpos = blk * QBLK + jnp.arange(QBLK)[None, :, None]
    kpos = (blk - 1) * QBLK + jnp.arange(3 * QBLK)[None, None, :]
    valid = (jnp.abs(kpos - qpos) <= WINDOW) & (kpos >= 0) & (kpos < s_len)
    s_loc = jnp.where(valid, s_loc, NEG_INF)
    s_ctx = jnp.einsum('bhgnqd,bhkd->bhgnqk', qb, k_ctx).astype(jnp.float32) * scale
    s_sink = jnp.broadcast_to(sink.astype(jnp.float32)[None, :, :, None, None, None], s_loc.shape[:-1] + (1,))
    p = jax.nn.softmax(jnp.concatenate([s_loc, s_ctx, s_sink], axis=-1), axis=-1)
    n_loc, n_ctx = 3 * QBLK, k_ctx.shape[2]
    o = (jnp.einsum('bhgnqk,bhnkd->bhgnqd', p[..., :n_loc].astype(v.dtype), vb)
         + jnp.einsum('bhgnqk,bhkd->bhgnqd', p[..., n_loc:n_loc + n_ctx].astype(v.dtype), v_ctx))
    return o.reshape(b, hkv, g, s_len, d)


def token_mixers(h_lat, h_ctx, rope128, rope64, lambda_init, ctx_out, w_in, qn_a, kn_a, qn_b, kn_b,
                 lam_q1, lam_k1, lam_q2, lam_k2, subln_b, qn_c, kn_c, sink_c,
                 w_br_a, w_br_b, w_br_c, w_out):
    p_lat = h_lat @ w_in
    p_ctx = h_ctx @ (w_in if ctx_out else w_in[:, :KV_W])
    kA_l, vA_l, kB_l, vB_l, kC_l, vC_l = jnp.split(p_lat[..., :KV_W], KV_SPLITS, axis=-1)
    kA_c, vA_c, kB_c, vB_c, kC_c, vC_c = jnp.split(p_ctx[..., :KV_W], KV_SPLITS, axis=-1)

    kA_cx = qk_heads(kA_c, A_KV_HEADS, HEAD_DIM, kn_a, None)
    vA_cx = v_heads(vA_c, A_KV_HEADS, HEAD_DIM)
    kB_cx = qk_heads(kB_c, 2 * B_HEADS, B_QK_DIM, kn_b, None)
    vB_cx = v_heads(vB_c, B_HEADS, B_V_DIM)
    kC_cx = qk_heads(kC_c, C_KV_HEADS, HEAD_DIM, kn_c, None)
    vC_cx = v_heads(vC_c, C_KV_HEADS, HEAD_DIM)

    kA = jnp.concatenate([qk_heads(kA_l, A_KV_HEADS, HEAD_DIM, kn_a, rope128), kA_cx], axis=2)
    vA = jnp.concatenate([v_heads(vA_l, A_KV_HEADS, HEAD_DIM), vA_cx], axis=2)
    kB1, kB2 = split_pairs(jnp.concatenate([qk_heads(kB_l, 2 * B_HEADS, B_QK_DIM, kn_b, rope64), kB_cx], axis=2))
    vB = jnp.concatenate([v_heads(vB_l, B_HEADS, B_V_DIM), vB_cx], axis=2)
    kC_lat = qk_heads(kC_l, C_KV_HEADS, HEAD_DIM, kn_c, rope128)
    vC_lat = v_heads(vC_l, C_KV_HEADS, HEAD_DIM)

    lam = (jnp.exp(jnp.sum(lam_q1.astype(jnp.float32) * lam_k1.astype(jnp.float32)))
           - jnp.exp(jnp.sum(lam_q2.astype(jnp.float32) * lam_k2.astype(jnp.float32))) + lambda_init)
    sink = sink_c.reshape(C_KV_HEADS, C_Q_HEADS // C_KV_HEADS)

    def queries(p, rope_a, rope_b):
        qA, qB, qC = jnp.split(p[..., KV_W:KV_W + Q_W], Q_SPLITS, axis=-1)
        qA = group_heads(qk_heads(qA, A_Q_HEADS, HEAD_DIM, qn_a, rope_a), A_KV_HEADS)
        qB1, qB2 = split_pairs(qk_heads(qB, 2 * B_HEADS, B_QK_DIM, qn_b, rope_b))
        qC = group_heads(qk_heads(qC, C_Q_HEADS, HEAD_DIM, qn_c, rope_a), C_KV_HEADS)
        gates = jax.nn.sigmoid(p[..., KV_W + Q_W:])
        return qA, qB1, qB2, qC, gates

    def merge(oA, oB, oC, gates):
        gA, gB, gC = jnp.split(gates, 3, axis=-1)
        oB = rms_norm(oB, subln_b) * (1.0 - lambda_init)
        m = (gA * (heads_to_tokens(oA) @ w_br_a) + gB * (heads_to_tokens(oB) @ w_br_b)
             + gC * (heads_to_tokens(oC) @ w_br_c))
        return m @ w_out

    qA, qB1, qB2, qC, g = queries(p_lat, rope128, rope64)
    oA = sweep_blocks(lambda q: gqa_block(q, kA, vA), (qA,))
    oB = sweep_blocks(lambda q1, q2: diff_block(q1, q2, kB1, kB2, vB, lam), (qB1, qB2))
    oC = window_attention(qC, kC_lat, vC_lat, kC_cx, vC_cx, sink)
    out_lat = merge(oA, oB, oC, g)
    if not ctx_out:
        return out_lat, None

    qA, qB1, qB2, qC, g = queries(p_ctx, None, None)
    kB1c, kB2c = split_pairs(kB_cx)
    oA = gqa_block(qA, kA_cx, vA_cx)
    oB = diff_block(qB1, qB2, kB1c, kB2c, vB_cx, lam)
    oC = gqa_block(qC, kC_cx, vC_cx, sink)
    out_ctx = merge(oA, oB, oC, g)
    return out_lat, out_ctx


def ec_moe(h, w_router, w_gate, w_up, w_down):
    b, n, _ = h.shape
    cap = EC_CAPACITY * n // N_EXPERTS
    aff = jax.nn.softmax((h @ w_router).astype(jnp.float32), axis=-1)
    g, idx = lax.top_k(aff.transpose(0, 2, 1), cap)
    xg = h[jnp.arange(b)[:, None], idx.reshape(b, -1)].reshape(b, N_EXPERTS, cap, -1)
    u = jnp.einsum('becd,edf->becf', xg, w_gate)
    v = jnp.einsum('becd,edf->becf', xg, w_up)
    y = jnp.einsum('becf,efd->becd', jax.nn.silu(u) * v, w_down) * g[..., None].astype(h.dtype)
    return jnp.zeros_like(h).at[jnp.arange(b)[:, None, None], idx].add(y)


def setup_inputs(seed: int = 0) -> dict:
    key = jax.random.key(seed)
    ks = jax.random.split(key, 32)

    def nrm(k, shape, scale):
        return jax.random.normal(k, shape, jnp.float32) * scale

    return {
        "x": nrm(ks[0], (BATCH, SEQ, D_MODEL), 1.0),
        "c": nrm(ks[1], (BATCH, D_MODEL), 1.0),
        "ctx": nrm(ks[2], (BATCH, CTX_LEN, D_MODEL), 1.0),
        "c_ctx": nrm(ks[3], (D_MODEL,), 1.0),
        "w_mod": nrm(ks[4], (DEPTH, D_MODEL, 6 * D_MODEL), 0.5 * D_MODEL ** -0.5),
        "b_mod": nrm(ks[5], (DEPTH, 6 * D_MODEL), 0.02),
        "norm_mix": 1.0 + nrm(ks[6], (DEPTH, D_MODEL), 0.02),
        "norm_ffn": 1.0 + nrm(ks[7], (DEPTH, D_MODEL), 0.02),
        "w_in": nrm(ks[8], (DEPTH, D_MODEL, IN_W), D_MODEL ** -0.5),
        "qn_a": 1.0 + nrm(ks[9], (DEPTH, HEAD_DIM), 0.02),
        "kn_a": 1.0 + nrm(ks[10], (DEPTH, HEAD_DIM), 0.02),
        "qn_b": 1.0 + nrm(ks[11], (DEPTH, B_QK_DIM), 0.02),
        "kn_b": 1.0 + nrm(ks[12], (DEPTH, B_QK_DIM), 0.02),
        "lam_q1": nrm(ks[13], (DEPTH, B_QK_DIM), 0.1),
        "lam_k1": nrm(ks[14], (DEPTH, B_QK_DIM), 0.1),
        "lam_q2": nrm(ks[15], (DEPTH, B_QK_DIM), 0.1),
        "lam_k2": nrm(ks[16], (DEPTH, B_QK_DIM), 0.1),
        "subln_b": 1.0 + nrm(ks[17], (DEPTH, B_V_DIM), 0.02),
        "qn_c": 1.0 + nrm(ks[18], (DEPTH, HEAD_DIM), 0.02),
        "kn_c": 1.0 + nrm(ks[19], (DEPTH, HEAD_DIM), 0.02),
        "sink_c": nrm(ks[20], (DEPTH, C_Q_HEADS), 0.5),
        "w_br_a": nrm(ks[21], (DEPTH, A_Q_W, D_MODEL), A_Q_W ** -0.5),
        "w_br_b": nrm(ks[22], (DEPTH, B_V_W, D_MODEL), B_V_W ** -0.5),
        "w_br_c": nrm(ks[23], (DEPTH, C_Q_W, D_MODEL), C_Q_W ** -0.5),
        "w_out": nrm(ks[24], (DEPTH, D_MODEL, D_MODEL), D_MODEL ** -0.5),
        "w_router": nrm(ks[25], (DEPTH, D_MODEL, N_EXPERTS), D_MODEL ** -0.5),
        "w_gate": nrm(ks[26], (DEPTH, N_EXPERTS, D_MODEL, EXPERT_FF), D_MODEL ** -0.5),
        "w_up": nrm(ks[27], (DEPTH, N_EXPERTS, D_MODEL, EXPERT_FF), D_MODEL ** -0.5),
        "w_down": nrm(ks[28], (DEPTH, N_EXPERTS, EXPERT_FF, D_MODEL), EXPERT_FF ** -0.5),
    }


def reference(x, c, ctx, c_ctx, w_mod, b_mod, norm_mix, norm_ffn, w_in, qn_a, kn_a, qn_b, kn_b,
              lam_q1, lam_k1, lam_q2, lam_k2, subln_b, qn_c, kn_c, sink_c,
              w_br_a, w_br_b, w_br_c, w_out, w_router, w_gate, w_up, w_down):
    s_len = x.shape[1]
    rows = s_len // GRID_W
    row = jnp.repeat(jnp.arange(rows, dtype=jnp.int32), GRID_W)
    col = jnp.arange(s_len, dtype=jnp.int32) % GRID_W
    rope128 = axial_rope_tables(row, col, HEAD_DIM, x.dtype)
    rope64 = axial_rope_tables(row, col, B_QK_DIM, x.dtype)
    sc = jax.nn.silu(c)
    scc = jax.nn.silu(c_ctx)
    xc = ctx
    for l in range(DEPTH):
        last = l == DEPTH - 1
        lambda_init = 0.8 - 0.6 * math.exp(-0.3 * l)
        mod = (sc @ w_mod[l] + b_mod[l])[:, None, :]
        sh1, s1, g1, sh2, s2, g2 = jnp.split(mod, 6, axis=-1)
        n_mod_ctx = 2 if last else 6
        mc = jnp.split(scc @ w_mod[l][:, :n_mod_ctx * D_MODEL] + b_mod[l][:n_mod_ctx * D_MODEL], n_mod_ctx)
        h_lat = modulate(rms_norm(x, norm_mix[l]), sh1, s1)
        h_ctx = modulate(rms_norm(xc, norm_mix[l]), mc[0], mc[1])
        o_lat, o_ctx = token_mixers(h_lat, h_ctx, rope128, rope64, lambda_init, not last, w_in[l],
                                    qn_a[l], kn_a[l], qn_b[l], kn_b[l], lam_q1[l], lam_k1[l],
                                    lam_q2[l], lam_k2[l], subln_b[l], qn_c[l], kn_c[l], sink_c[l],
                                    w_br_a[l], w_br_b[l], w_br_c[l], w_out[l])
        x = x + g1 * o_lat
        x = x + g2 * ec_moe(modulate(rms_norm(x, norm_ffn[l]), sh2, s2),
                            w_router[l], w_gate[l], w_up[l], w_down[l])
        if not last:
            xc = xc + mc[2] * o_ctx
            xc = xc + mc[5] * ec_moe(modulate(rms_norm(xc, norm_ffn[l]), mc[3], mc[4]),
                                     w_router[l], w_gate[l], w_up[l], w_down[l])
    return x
```

```python
import contextlib
import math
import numpy as np
import ml_dtypes
import concourse.bass as bass
import concourse.mybir as mybir
from concourse.bass_utils import run_bass_kernel_spmd

F32 = mybir.dt.float32
BF16 = mybir.dt.bfloat16
I32 = mybir.dt.int32
U32 = mybir.dt.uint32
ALU = mybir.AluOpType
AF = mybir.ActivationFunctionType
AX = mybir.AxisListType
NPBF = ml_dtypes.bfloat16

D = 2048
SEQ = 4096
CTX = 256
NCORE = 8
TL = 1024
TC = 64
TT = TL + TC
KEYS = SEQ + CTX
NKB = KEYS // 128
IN_W = 10240
EPS = 1e-6
NEXP = 16
FF = 1024
CAP_L = 512
CAP_C = 32

ENGS = ("sync", "scalar", "vector", "gpsimd", "tensor")
NDMA_SEM = 12
CC_OVERLAP = False
KV_SPREAD = True


class Prog:
    def __init__(self, nc):
        self.nc = nc
        self.q = {e: [] for e in ENGS}
        self.cnt = {e: 0 for e in ENGS}
        self.waited = {e: {} for e in ENGS}
        self.res = {}
        self.dma_i = {e: 0 for e in ENGS}
        self.dma_cnt = {}
        self.final_tokens = []

    def barrier(self):
        bar = [(("eng", e), c) for e, c in self.cnt.items() if c > 0]
        for key, c in self.dma_cnt.items():
            bar.append((key, c if key[0] == "cc" else 16 * c))
        self.bar = bar

    def op(self, eng, fn, reads=(), writes=(), dma=False, final=False, cc=False):
        deps = list(getattr(self, "bar", ()))
        for r in reads:
            st = self.res.get(r)
            if st and st["w"]:
                deps.append(st["w"])
        for r in writes:
            st = self.res.get(r)
            if st:
                if st["w"]:
                    deps.append(st["w"])
                deps.extend(st["r"].items())
        if cc:
            key = ("cc", eng, 0)
            c = self.dma_cnt.get(key, 0)
            if c > 0 and not CC_OVERLAP:
                deps.append((key, c))
            self.dma_cnt[key] = c + 1
            token = (key, c + 1)
            inc = 1
        elif dma:
            j = self.dma_i[eng] % NDMA_SEM
            self.dma_i[eng] += 1
            key = ("dma", eng, j)
            c = self.dma_cnt.get(key, 0)
            if c > 0:
                deps.append((key, 16 * c))
            self.dma_cnt[key] = c + 1
            token = (key, 16 * (c + 1))
            inc = 16
        else:
            self.cnt[eng] += 1
            key = ("eng", eng)
            token = (key, self.cnt[eng])
            inc = 1
        waits = {}
        for (k, v) in deps:
            if k == ("eng", eng) and eng == "tensor":
                continue
            if self.waited[eng].get(k, 0) >= v:
                continue
            if waits.get(k, 0) < v:
                waits[k] = v
        for k, v in waits.items():
            self.waited[eng][k] = v
        self.q[eng].append((list(waits.items()), fn, key, inc))
        for r in reads:
            st = self.res.setdefault(r, {"w": None, "r": {}})
            if st["r"].get(token[0], 0) < token[1]:
                st["r"][token[0]] = token[1]
        for r in writes:
            self.res[r] = {"w": token, "r": {}}
        if final:
            self.final_tokens.append(token)
        return token

    def emit(self):
        nc = self.nc
        keys = set()
        for e in ENGS:
            for (w, fn, key, inc) in self.q[e]:
                keys.add(key)
        with contextlib.ExitStack() as es:
            sems = {}
            for k in sorted(keys):
                sems[k] = es.enter_context(nc.semaphore("s_" + "_".join(str(x) for x in k)))
            block = es.enter_context(nc.Block())
            finals = self.final_tokens

            def make(ename):
                def body(engine):
                    for (w, fn, key, inc) in self.q[ename]:
                        for (k, v) in w:
                            engine.wait_ge(sems[k], v)
                        ins = fn(engine)
                        ins.then_inc(sems[key], inc)
                    if ename == "sync":
                        for (k, v) in finals:
                            engine.wait_ge(sems[k], v)
                return body

            for e in ENGS:
                if self.q[e] or e == "sync":
                    getattr(block, e)(make(e))


ARENA_B = 204800
_ISZ = {F32: 4, U32: 4, I32: 4, BF16: 2}


class State:
    def __init__(self, nc, es):
        self.nc = nc
        self.P = Prog(nc)
        self.arena = es.enter_context(nc.sbuf_tensor("arena", [128, ARENA_B // 2], BF16))
        self.psum = es.enter_context(nc.psum_tensor("psum", [128, 4096], F32))
        self.v = {BF16: self.arena[:], F32: self.arena[:].bitcast(F32), U32: self.arena[:].bitcast(U32)}
        self.pv = {F32: self.psum[:], BF16: self.psum[:].bitcast(BF16)}
        self.off = 0
        self.poff = 0
        self.nd = 0

    def phase(self):
        self.off = 0
        self.poff = 0
        self.P.barrier()

    @staticmethod
    def _shape_view(view, shape):
        if len(shape) == 2:
            return view
        names = ["a%d" % i for i in range(len(shape) - 1)]
        kw = {n: s for n, s in zip(names, shape[1:])}
        return view.rearrange("p (%s) -> p %s" % (" ".join(names), " ".join(names)), **kw)

    def sb(self, shape, dt, name=None):
        isz = _ISZ[dt]
        n = int(np.prod(shape[1:]))
        nb = (n * isz + 31) // 32 * 32
        assert self.off + nb <= ARENA_B, ("SBUF arena overflow", self.off, nb)
        o = self.off // isz
        self.off += nb
        return self._shape_view(self.v[dt][:shape[0], o:o + n], list(shape))

    def ps(self, shape, dt, name=None):
        isz = _ISZ[dt]
        n = int(np.prod(shape[1:]))
        nb = (n * isz + 2047) // 2048 * 2048
        assert self.poff + nb <= 16384, "PSUM overflow"
        o = self.poff // isz
        self.poff += nb
        return self._shape_view(self.pv[dt][:shape[0], o:o + n], list(shape))

    def dram(self, shape, dt, name=None):
        self.nd += 1
        return self.nc.dram_tensor(name or ("scr%d" % self.nd), list(shape), dt).ap()


def _din(nc, name, shape, dt):
    return nc.dram_tensor(name, list(shape), dt, kind="ExternalInput").ap()


def _dout(nc, name, shape, dt):
    return nc.dram_tensor(name, list(shape), dt, kind="ExternalOutput").ap()


RG = [[0, 1, 2, 3], [4, 5, 6, 7]]


def _cc(P, kind, op, src, dst, reads, writes):
    P.op("gpsimd", lambda e: e.collective_compute(kind, op, replica_groups=RG, ins=[src.opt()], outs=[dst.opt()]),
         reads=reads, writes=writes, cc=True)


MW = 6144


def phase_M(S, T):
    P = S.P
    S.phase()
    ct = S.sb([128, 16, 2], F32)
    st = S.sb([128, 16, 2], F32)
    bt = S.sb([2, MW], F32)
    ot = S.sb([2, MW], F32)
    ws = [S.sb([128, 16, 512], F32) for _ in range(2)]
    pm = [S.ps([128, 512], F32) for _ in range(2)]
    P.op("sync", lambda e: e.dma_start(out=ct[:], in_=T["cT"]), writes=["ct"], dma=True)
    P.op("sync", lambda e: e.dma_start(out=bt[:], in_=T["bm"].partition_broadcast(2)), writes=["bt"], dma=True)
    P.op("scalar", lambda e: e.activation(out=st[:], in_=ct[:], func=AF.Silu), reads=["ct"], writes=["st"])
    for g in range(MW // 512):
        s = g % 2
        P.op("sync" if g % 2 == 0 else "gpsimd",
             lambda e, g=g, s=s: e.dma_start(out=ws[s][:], in_=T["wm"][:, g * 512:(g + 1) * 512].rearrange("(k p) c -> p k c", p=128)),
             writes=["ws%d" % s], dma=True)
        for k in range(16):
            P.op("tensor", lambda e, k=k, s=s: e.matmul(pm[s][0:2, :], lhsT=st[:, k, :], rhs=ws[s][:, k, :], start=(k == 0), stop=(k == 15)),
                 reads=["st", "ws%d" % s], writes=["pm%d" % s])
        P.op("vector", lambda e, g=g, s=s: e.tensor_tensor(out=ot[:, g * 512:(g + 1) * 512], in0=pm[s][0:2, :], in1=bt[:, g * 512:(g + 1) * 512], op=ALU.add),
             reads=["pm%d" % s, "bt"], writes=["ot"])
    P.op("sync", lambda e: e.dma_start(out=T["modloc"], in_=ot[:]), reads=["ot"], writes=["modloc"], dma=True)
    _cc(P, "AllGather", ALU.bypass, T["modloc"], T["modG"], ["modloc"], ["modG"])


def modv(T, l, r, ch):
    row = (2 * l + ch // 3) * 2 + r
    c0 = (ch % 3) * D
    return T["modG"][row, c0:c0 + D]


SEGS = {
    0: [(0, 256, "n128", "KTA", 0, 1), (256, 256, "v", 0)],
    1: [(0, 512, "n64", "KTB", 0, 1)],
    2: [(0, 512, "v", 256)],
    3: [(0, 256, "n128", "KTC", 0, 3), (256, 256, "v", 768)],
    4: [(0, 512, "n128", "QTA", 0, 0)],
    5: [(0, 256, "n128", "QTA", 4, 0), (256, 256, "n64", "QTB", 0, 0)],
    6: [(0, 256, "n64", "QTB", 4, 0), (256, 256, "n128", "QTC", 0, 2)],
    7: [(0, 512, "n128", "QTC", 2, 2)],
}
DEST = {"KTA": ("K0", 0), "KTC": ("K0", 256), "KTB": ("K1", 0), "QTA": ("Q", 0), "QTB": ("Q", 768), "QTC": ("Q", 1280)}


def phase_A(S, T, l, combine, proj, nt, xsrc, xdst, final_out=False):
    P = S.P
    S.phase()
    L = T["L"][l] if proj else None
    if combine:
        Lp = T["L"][l - 1]

    def rows(t):
        return 128 if t < 8 else TC

    xt = [S.sb([128, D], F32) for _ in range(2)]
    if combine:
        acc = S.sb([128, D], F32)
        g2t = S.sb([128, D], F32)
    if proj:
        amul = S.sb([128, D], F32)
        bsh = S.sb([128, D], F32)
        sqs = S.sb([128, D], F32)
        ss = S.sb([128, 2], F32)
        epst = S.sb([128, 1], F32)
        hb = [S.sb([128, D], BF16) for _ in range(2)]
        hT = S.sb([128, 16, TT], BF16)
        idt = S.sb([128, 128], BF16)
        gt128 = S.sb([128, 4, 128], F32)
        gt64 = S.sb([128, 2, 64], F32)
        c128 = S.sb([128, 8, 128], F32)
        s128 = S.sb([128, 8, 128], F32)
        c64 = S.sb([128, 8, 64], F32)
        s64 = S.sb([128, 8, 64], F32)
        wsl = [S.sb([128, 16, 512], BF16) for _ in range(2)]
        NS = 4
        sq2 = [S.sb([128, 512], F32) for _ in range(NS)]
        ssh = [S.sb([128, 8], F32) for _ in range(NS)]
        yf = [S.sb([128, 512], F32) for _ in range(NS)]
        t1 = [S.sb([128, 512], F32) for _ in range(NS)]
        t2 = [S.sb([128, 512], F32) for _ in range(NS)]
        yb = [S.sb([128, 512], BF16) for _ in range(NS)]
        qk = [S.sb([128, 8, 128], BF16) for _ in range(4)]
        ob = [S.sb([128, 512], BF16) for _ in range(6)]
        NPA = 5
        pacc = [S.ps([128, 512], F32) for _ in range(NPA)]
        pT = S.pv[BF16][:, 0:2048].rearrange("p (k t) -> p k t", k=16)
        pTh = [S.ps([128, 8, 128], BF16) for _ in range(2)]
        P.op("sync", lambda e: e.dma_start(out=idt[:], in_=T["ident"]), writes=["idt"], dma=True)
        P.op("sync", lambda e: e.dma_start(out=gt128[:], in_=T["g128"][l].partition_broadcast(128)), writes=["gt128"], dma=True)
        P.op("sync", lambda e: e.dma_start(out=gt64[:], in_=T["g64"][l].partition_broadcast(128)), writes=["gt64"], dma=True)
        for (dst, srcn, nm) in ((c128, "cos128", "c128"), (s128, "sin128", "s128"), (c64, "cos64", "c64"), (s64, "sin64", "s64")):
            P.op("sync", lambda e, dst=dst, srcn=srcn: e.dma_start(out=dst[:], in_=T[srcn].rearrange("(t p) d -> p t d", p=128)), writes=[nm], dma=True)
        P.op("vector", lambda e: e.memset(epst[:], EPS), writes=["epst"])

    def load_mod(which):
        P.op("sync", lambda e: e.dma_start(out=amul[:], in_=modv(T, l, which, 1).partition_broadcast(128)), writes=["amul"], dma=True)
        P.op("sync", lambda e: e.dma_start(out=bsh[:], in_=T["norm_mix"][l].partition_broadcast(128)), writes=["bsh"], dma=True)
        P.op("vector", lambda e: e.scalar_tensor_tensor(out=amul[:], in0=amul[:], scalar=1.0, in1=bsh[:], op0=ALU.add, op1=ALU.mult),
             reads=["amul", "bsh"], writes=["amul"])
        P.op("sync", lambda e: e.dma_start(out=bsh[:], in_=modv(T, l, which, 0).partition_broadcast(128)), reads=["bsh"], writes=["bsh"], dma=True)

    def load_g2(which):
        P.op("sync", lambda e: e.dma_start(out=g2t[:], in_=modv(T, l - 1, which, 5).partition_broadcast(128)), writes=["g2t"], dma=True)

    if proj:
        load_mod(0)
    if combine:
        load_g2(0)
    for t in range(nt):
        r = rows(t)
        r0 = t * 128
        xs = t % 2
        xk = "xt%d" % xs
        if t == 8:
            if proj:
                load_mod(1)
            if combine:
                load_g2(1)
        P.op("sync", lambda e, xs=xs, r=r, r0=r0: e.dma_start(out=xt[xs][:r, :], in_=xsrc[r0:r0 + r, :]), writes=[xk], dma=True)
        if combine:
            csrc = Lp["comb%d" % (t // 2)][(t % 2) * 128:(t % 2) * 128 + r, :] if t < 8 else Lp["comb_ctx"][0:r, :]
            P.op("gpsimd", lambda e, r=r, csrc=csrc: e.dma_start(out=acc[:r, :], in_=csrc), writes=["acc"], dma=True)
            P.op("gpsimd", lambda e, r=r: e.tensor_tensor(out=acc[:r, :], in0=acc[:r, :], in1=g2t[:r, :], op=ALU.mult), reads=["acc", "g2t"], writes=["acc"])
            P.op("vector", lambda e, xs=xs, r=r: e.tensor_tensor(out=xt[xs][:r, :], in0=xt[xs][:r, :], in1=acc[:r, :], op=ALU.add), reads=["acc", xk], writes=[xk])
            P.op("sync", lambda e, xs=xs, r=r, r0=r0: e.dma_start(out=xdst[r0:r0 + r, :], in_=xt[xs][:r, :]), reads=[xk], dma=True, final=final_out)
        if not proj:
            continue
        hs = t % 2
        hk = "hb%d" % hs
        P.op("scalar", lambda e, xs=xs, hs=hs, r=r: e.activation(out=hb[hs][:r, :], in_=xt[xs][:r, :], func=AF.Square, accum_out=ss[:r, 0:1]),
             reads=[xk], writes=[hk, "ss"])
        P.op("scalar", lambda e, r=r: e.activation(out=ss[:r, 1:2], in_=ss[:r, 0:1], func=AF.Sqrt, bias=epst[:r, :], scale=1.0 / D),
             reads=["ss", "epst"], writes=["ss1"])
        P.op("vector", lambda e, r=r: e.reciprocal(out=ss[:r, 1:2], in_=ss[:r, 1:2]), reads=["ss1"], writes=["ss1"])
        P.op("vector", lambda e, xs=xs, r=r: e.scalar_tensor_tensor(out=sqs[:r, :], in0=xt[xs][:r, :], scalar=ss[:r, 1:2], in1=amul[:r, :], op0=ALU.mult, op1=ALU.mult),
             reads=[xk, "ss1", "amul", "sqs"], writes=["sqs"])
        P.op("gpsimd", lambda e, hs=hs, r=r: e.tensor_tensor(out=hb[hs][:r, :], in0=sqs[:r, :], in1=bsh[:r, :], op=ALU.add),
             reads=["sqs", "bsh"], writes=[hk])
        for k in range(16):
            P.op("tensor", lambda e, k=k, hs=hs, r=r: e.transpose(out=pT[:, k, :r], in_=hb[hs][:r, k * 128:(k + 1) * 128], identity=idt[:r, :r]),
                 reads=[hk, "idt"], writes=["pacc%d" % (k // 8)])
        P.op("scalar", lambda e, r=r, r0=r0: e.copy(out=hT[:, :, r0:r0 + r], in_=pT[:, :, :r]), reads=["pacc0", "pacc1"], writes=["hT%d" % t])

    if not proj:
        return

    w_in = T["w_in"][l]
    u = 0
    nseg = 0
    pending = []
    KVK = ["ccKV%d" % j_ for j_ in range(7)]
    TAILC = [0]
    ZC = [0]
    def load_w(g):
        s_ = g % 2
        P.op("gpsimd", lambda e, g=g, s_=s_: e.dma_start(out=wsl[s_][:], in_=w_in[:, g * 512:(g + 1) * 512].rearrange("(k p) c -> p k c", p=128)),
             writes=["wsl%d" % s_], dma=True)

    KVPAIRS = (("KTloc0", "KTG0"), ("Vloc0", "VG0"), ("KTloc1", "KTG1"), ("Vloc1", "VG1"), ("WKVloc", "WG"), ("KTlocC", "KTGc"), ("VlocC", "VGc"))

    def share_kv():
        for j_, (a, b) in enumerate(KVPAIRS):
            _cc(P, "AllGather", ALU.bypass, L[a], L[b], [], [b, KVK[j_]])

    load_w(0)
    for g in range(20):
        s = g % 2
        wk = "wsl%d" % s
        if g + 1 < 20:
            load_w(g + 1)
        if KV_SPREAD and 4 <= g < 4 + len(KVPAIRS):
            if g == 4:
                while pending:
                    for f_ in pending.pop(0):
                        f_()
            a_, b_ = KVPAIRS[g - 4]
            _cc(P, "AllGather", ALU.bypass, L[a_], L[b_], [], [b_, KVK[g - 4]])
        for t in range(nt):
            r = rows(t)
            r0 = t * 128
            lat = t < 8
            n_this = sum(1 for sg in SEGS.get(g, []) if sg[2] != "v")
            while pending and sum(len(x) for x in pending) + n_this > NS:
                for f_ in pending.pop(0):
                    f_()
            pb = u % NPA
            u += 1
            pk = "pacc%d" % pb
            for k in range(16):
                P.op("tensor", lambda e, k=k, s=s, pb=pb, r=r, r0=r0: e.matmul(pacc[pb][:r, :], lhsT=hT[:, k, r0:r0 + r], rhs=wsl[s][:, k, :], start=(k == 0), stop=(k == 15)),
                     reads=["hT%d" % t, wk], writes=[pk])
            unit_tails = []
            if g >= 8:
                o = nseg % 6
                nseg += 1
                ok = "ob%d" % o
                P.op("scalar", lambda e, pb=pb, o=o, r=r: e.activation(out=ob[o][:r, :], in_=pacc[pb][:r, :], func=AF.Sigmoid), reads=[pk], writes=[ok])
                P.op("sync", lambda e, o=o, r=r, r0=r0, g=g: e.dma_start(out=L["G"][r0:r0 + r, (g - 8) * 512:(g - 7) * 512], in_=ob[o][:r, :]), reads=[ok], dma=True)
                if pending:
                    for f_ in pending.pop(0):
                        f_()
                continue
            for seg in SEGS[g]:
                c0, w, kind = seg[0], seg[1], seg[2]
                if kind == "v":
                    o = nseg % 6
                    nseg += 1
                    ok = "ob%d" % o
                    vc = seg[3]
                    P.op("scalar", lambda e, pb=pb, o=o, r=r, c0=c0, w=w: e.copy(out=ob[o][:r, :w], in_=pacc[pb][:r, c0:c0 + w]), reads=[pk], writes=[ok])
                    vdst = L["Vloc%d" % (t // 4)][(t % 4) * 128:(t % 4) * 128 + r, vc:vc + w] if lat else L["VlocC"][0:r, vc:vc + w]
                    P.op("sync", lambda e, o=o, r=r, w=w, vdst=vdst: e.dma_start(out=vdst, in_=ob[o][:r, :w]), reads=[ok] + KVK, dma=True)
                    if vc == 768 and lat:
                        P.op("sync", lambda e, o=o, r=r, r0=r0, w=w: e.dma_start(out=L["WKVloc"][r0:r0 + r, 0:256], in_=ob[o][:r, :w]), reads=[ok] + KVK, dma=True)
                    continue
                dest, h0, gi = seg[3], seg[4], seg[5]
                d = 128 if kind == "n128" else 64
                H = w // d
                z = ZC[0] % NS
                ZC[0] += 1
                nseg += 1
                zk = "z%d" % z
                pv = pacc[pb][:r, c0:c0 + w].rearrange("p (h d) -> p h d", d=d)
                P.op("scalar", lambda e, pb=pb, z=z, r=r, c0=c0, w=w: e.activation(out=sq2[z][:r, :w], in_=pacc[pb][:r, c0:c0 + w], func=AF.Square),
                     reads=[pk], writes=[zk + "sq"])
                P.op("vector", lambda e, z=z, r=r, w=w, H=H, d=d: e.tensor_reduce(out=ssh[z][:r, :H], in_=sq2[z][:r, :w].rearrange("p (h d) -> p h d", d=d), axis=AX.X, op=ALU.add),
                     reads=[zk + "sq"], writes=[zk + "ss"])
                P.op("scalar", lambda e, z=z, r=r, H=H, d=d: e.activation(out=ssh[z][:r, :H], in_=ssh[z][:r, :H], func=AF.Sqrt, bias=epst[:r, :], scale=1.0 / d),
                     reads=[zk + "ss", "epst"], writes=[zk + "ss"])
                P.op("vector", lambda e, z=z, r=r, H=H: e.reciprocal(out=ssh[z][:r, :H], in_=ssh[z][:r, :H]), reads=[zk + "ss"], writes=[zk + "ss"])
                y3 = yf[z][:r, :w].rearrange("p (h d) -> p h d", d=d)
                P.op("vector", lambda e, pv=pv, y3=y3, z=z, r=r, H=H, d=d: e.tensor_tensor(out=y3, in0=pv, in1=ssh[z][:r, :H].unsqueeze(2).to_broadcast([r, H, d]), op=ALU.mult),
                     reads=[pk, zk + "ss"], writes=[zk + "y"])
                gtile = (gt128[:r, gi, :] if d == 128 else gt64[:r, gi, :])
                if lat:
                    P.op("vector", lambda e, y3=y3, gtile=gtile, r=r, H=H, d=d: e.tensor_tensor(out=y3, in0=y3, in1=gtile.unsqueeze(1).to_broadcast([r, H, d]), op=ALU.mult),
                         reads=[zk + "y", "gt128", "gt64"], writes=[zk + "y"])
                    ct_, st_ = (c128, s128) if d == 128 else (c64, s64)
                    e4 = d // 4
                    t13 = t1[z][:r, :w].rearrange("p (h d) -> p h d", d=d)
                    P.op("vector", lambda e, y3=y3, t13=t13, ct_=ct_, t=t, r=r, H=H, d=d: e.tensor_tensor(out=t13, in0=y3, in1=ct_[:r, t, :].unsqueeze(1).to_broadcast([r, H, d]), op=ALU.mult),
                         reads=[zk + "y", "c128", "c64"], writes=[zk + "t1"])
                    y5 = yf[z][:r, :w].rearrange("p (h a b e) -> p h a b e", h=H, a=2, b=2, e=e4)
                    t25 = t2[z][:r, :w].rearrange("p (h a b e) -> p h a b e", h=H, a=2, b=2, e=e4)
                    s4 = st_[:r, t, :].rearrange("p (a b e) -> p a b e", a=2, b=2, e=e4)
                    for bsel in (0, 1):
                        P.op("vector",
                             lambda e, y5=y5, t25=t25, s4=s4, bsel=bsel, r=r, H=H, e4=e4: e.tensor_tensor(out=t25[:, :, :, bsel, :], in0=y5[:, :, :, 1 - bsel, :], in1=s4[:, :, bsel, :].unsqueeze(1).to_broadcast([r, H, 2, e4]), op=ALU.mult),
                             reads=[zk + "y", "s128", "s64"], writes=[zk + "t2%d" % bsel])
                    P.op("vector", lambda e, z=z, r=r, w=w: e.tensor_tensor(out=yb[z][:r, :w], in0=t1[z][:r, :w], in1=t2[z][:r, :w], op=ALU.add),
                         reads=[zk + "t1", zk + "t20", zk + "t21"], writes=[zk + "yb"])
                    if dest == "KTC":
                        P.op("sync", lambda e, z=z, r=r, r0=r0, w=w: e.dma_start(out=L["WKVloc"][r0:r0 + r, 256:512], in_=yb[z][:r, :w]), reads=[zk + "yb"] + KVK, dma=True)
                else:
                    yb3 = yb[z][:r, :w].rearrange("p (h d) -> p h d", d=d)
                    P.op("vector", lambda e, y3=y3, yb3=yb3, gtile=gtile, r=r, H=H, d=d: e.tensor_tensor(out=yb3, in0=y3, in1=gtile.unsqueeze(1).to_broadcast([r, H, d]), op=ALU.mult),
                         reads=[zk + "y", "gt128", "gt64"], writes=[zk + "yb"])
                kind_, base = DEST[dest]
                f0 = base + h0 * d
                if kind_ == "Q":
                    dd = L["QT"][f0:f0 + H * d, r0:r0 + r]
                elif lat:
                    dd = L["KTloc0" if kind_ == "K0" else "KTloc1"][f0:f0 + H * d, r0:r0 + r]
                else:
                    fc = f0 + (512 if kind_ == "K1" else 0)
                    dd = L["KTlocC"][fc:fc + H * d, 0:r]

                def tail(z=z, zk=zk, r=r, H=H, d=d, dd=dd, isk=(kind_ != "Q")):
                    tc_ = TAILC[0]
                    TAILC[0] += 1
                    ph = tc_ % 2
                    phk = "pTh%d" % ph
                    for j in range(H):
                        P.op("tensor", lambda e, j=j: e.transpose(out=pTh[ph][:d, j, :r], in_=yb[z][:r, j * d:(j + 1) * d], identity=idt[:r, :r]),
                             reads=[zk + "yb", "idt"], writes=[phk])
                    qs = tc_ % 4
                    qkk = "qk%d" % qs
                    P.op("scalar", lambda e: e.copy(out=qk[qs][:d, :H, :r], in_=pTh[ph][:d, :H, :r]), reads=[phk], writes=[qkk])
                    P.op("sync", lambda e: e.dma_start(out=dd.rearrange("(h d) t -> d h t", d=d), in_=qk[qs][:d, :H, :r]), reads=[qkk] + (KVK if isk else []), dma=True)

                unit_tails.append(tail)
            pending.append(unit_tails)
    for ut in pending:
        for f_ in ut:
            f_()
    if not KV_SPREAD:
        S.P.barrier()
        share_kv()
ARENA = 36864
ACC_DEN = False


def phase_B(S, T, l, ntiles, xres):
    layer = l
    lam_init = 0.8 - 0.6 * math.exp(-0.3 * layer)
    has_ctx = ntiles == 9
    ntq = TL + (TC if has_ctx else 0)
    P = S.P
    S.phase()
    L = T["L"][l]
    Gin = L["G"]
    xin = xres
    w_br_a, w_br_b, w_br_c, w_out, w_router = T["w_br_a"][l], T["w_br_b"][l], T["w_br_c"][l], T["w_out"][l], T["w_router"][l]
    normf = T["norm_ffn"][l]
    lamv, subln, sink = T["lamv"][l], T["subln"][l], T["sink"][l]
    ident, identf, bandm = T["ident"], T["identf"], T["bandm"]
    x1o = L["x1"]
    KTG0, KTG1, KTGc, VG0, VG1, VGc, WG, QT = L["KTG0"], L["KTG1"], L["KTGc"], L["VG0"], L["VG1"], L["VGc"], L["WG"], L["QT"]
    VGs = [VG0, VG1]

    def rows(t):
        return 128 if t < 8 else TC

    if True:
        C = S
        abase = S.off
        arena = S.sb([128, ARENA], BF16)
        qar = C.sb([128, 6 * ntq], BF16)
        OT = C.sb([128, 16, ntq], BF16)
        ptl = [C.sb([128, 512], BF16) for _ in range(6)]
        rec = C.sb([128, 512], F32)
        oa = C.sb([128, 512], F32)
        obb = C.sb([128, 512], F32)
        od = C.sb([128, 512], F32)
        osq = C.sb([128, 512], BF16)
        ones = C.sb([128, 128], BF16)
        onesf = C.sb([128, 128], F32)
        accD = [[C.sb([128, 512], F32) for _ in range(2)] for _ in range(2)]
        idt = C.sb([128, 128], BF16)
        idf = C.sb([128, 128], F32)
        bm = C.sb([128, 8, 2, 128], BF16)
        widx = C.sb([128, 10], U32)
        tl = C.sb([128, 4, 64], F32)
        tl2 = C.sb([128, 2, 64], F32)
        lsc = C.sb([128, 4], F32)
        subs = C.sb([128, 1], F32)
        esink = C.sb([128, 6], F32)
        epst = C.sb([128, 1], F32)
        fsc = C.sb([128, 1], F32)
        wr = C.sb([128, 16, NEXP], F32)
        g1t = [C.sb([128, D], F32) for _ in range(2)]
        xsl = [C.sb([128, 512], F32) for _ in range(2)]
        gsl = [C.sb([128, 3, 512], BF16) for _ in range(2)]
        mtmp = [C.sb([128, 512], F32) for _ in range(2)]
        mb = [C.sb([128, 512], BF16) for _ in range(2)]
        sm = C.sb([128, 4], F32)
        afft = [C.sb([128, NEXP], F32) for _ in range(2)]
        affTs = [C.sb([16, 128], F32) for _ in range(2)]
        bank = [C.ps([128, 512], F32) for i in range(8)]
        pTb = S.pv[BF16][:, 7 * 1024:7 * 1024 + 512].rearrange("p (c t) -> p c t", c=4)

        P.op("sync", lambda e: e.dma_start(out=idt[:], in_=ident), writes=["idt"], dma=True)
        P.op("sync", lambda e: e.dma_start(out=idf[:], in_=identf), writes=["idf"], dma=True)
        P.op("sync", lambda e: e.dma_start(out=bm[:], in_=bandm), writes=["bm"], dma=True)
        P.op("sync", lambda e: e.dma_start(out=widx[:], in_=T["widx"]), writes=["widx"], dma=True)
        P.op("sync", lambda e: e.dma_start(out=tl[:], in_=lamv.partition_broadcast(128)), writes=["tl"], dma=True)
        P.op("sync", lambda e: e.dma_start(out=subs[:], in_=subln.rearrange("(p o) -> p o", o=1)), writes=["subs"], dma=True)
        P.op("sync", lambda e: e.dma_start(out=esink[:], in_=sink.partition_broadcast(128)), writes=["esink"], dma=True)
        P.op("sync", lambda e: e.dma_start(out=wr[:], in_=w_router.rearrange("(k p) n -> p k n", p=128)), writes=["wr"], dma=True)
        for w_ in (0, 1):
            P.op("sync", lambda e, w_=w_: e.dma_start(out=g1t[w_][:], in_=modv(T, l, w_, 2).partition_broadcast(128)), writes=["g1t%d" % w_], dma=True)
        P.op("vector", lambda e: e.memset(ones[:], 1.0), writes=["ones"])
        P.op("vector", lambda e: e.memset(onesf[:], 1.0), writes=["onesf"])
        P.op("vector", lambda e: e.memset(epst[:], EPS), writes=["epst"])
        P.op("vector", lambda e: e.memset(fsc[:], 0.0), writes=["fsc"])
        tl4 = tl[:].rearrange("p (a b) d -> p a b d", b=2)
        P.op("vector", lambda e: e.tensor_tensor(out=tl2[:], in0=tl4[:, :, 0, :], in1=tl4[:, :, 1, :], op=ALU.mult), reads=["tl"], writes=["tl2"])
        P.op("vector", lambda e: e.tensor_reduce(out=lsc[:, 0:2], in_=tl2[:], axis=AX.X, op=ALU.add), reads=["tl2"], writes=["lsc"])
        P.op("scalar", lambda e: e.activation(out=lsc[:, 0:2], in_=lsc[:, 0:2], func=AF.Exp), reads=["lsc"], writes=["lsc"])
        P.op("vector", lambda e: e.tensor_tensor(out=lsc[:, 2:3], in0=lsc[:, 1:2], in1=lsc[:, 0:1], op=ALU.subtract), reads=["lsc"], writes=["lsc2"])
        P.op("vector", lambda e: e.tensor_scalar(out=lsc[:, 3:4], in0=lsc[:, 2:3], scalar1=-lam_init, scalar2=None, op0=ALU.add), reads=["lsc2"], writes=["neglam"])
        P.op("vector", lambda e: e.tensor_scalar(out=subs[:], in0=subs[:], scalar1=1.0 - lam_init, scalar2=None, op0=ALU.mult), reads=["subs"], writes=["subs"])
        P.op("scalar", lambda e: e.activation(out=esink[:], in_=esink[:], func=AF.Exp), reads=["esink"], writes=["esink"])

        arena_keys = []

        def fence(new_keys):
            P.op("gpsimd", lambda e: e.memset(fsc[:], 0.0), writes=["fsc"] + arena_keys + new_keys)
            arena_keys[:] = list(new_keys)

        state = {"psi": 0, "pti": 0, "ui": 0}

        def run_units(units):
            steps = []
            for u in units:
                u["uid"] = state["ui"]
                state["ui"] += 1
                nk = len(u["kblocks"])
                u["accm"] = ACC_DEN and nk >= 8
                u["nacc"] = 0
                for i, kb in enumerate(u["kblocks"]):
                    steps.append((u, kb, i == 0, i == nk - 1))
            LA = 3
            slots = {}
            for i in range(len(steps) + LA):
                if i < len(steps):
                    u, (kT, v, mask), first, last = steps[i]
                    nq = u["nq"]
                    b = state["psi"] % 4
                    state["psi"] += 1
                    p = state["pti"] % 6
                    state["pti"] += 1
                    slots[i] = p
                    bk = "bank%d" % b
                    P.op("tensor", lambda e, kT=kT, q=u["q"], b=b, nq=nq: e.matmul(bank[b][:, :nq], lhsT=kT, rhs=q, start=True, stop=True),
                         reads=u["rk"], writes=[bk])
                    P.op("scalar", lambda e, b=b, p=p, nq=nq, sc=u["scale"]: e.activation(out=ptl[p][:, :nq], in_=bank[b][:, :nq], func=AF.Exp, scale=sc),
                         reads=[bk], writes=["ptl%d" % p])
                    if mask is not None:
                        P.op("vector", lambda e, p=p, nq=nq, mask=mask: e.tensor_tensor(out=ptl[p][:, :nq], in0=ptl[p][:, :nq], in1=mask, op=ALU.mult),
                             reads=["ptl%d" % p, "bm"], writes=["ptl%d" % p])
                    if u.get("accm"):
                        si = u["nacc"]
                        u["nacc"] += 1
                        ub_ = u["uid"] % 2
                        dst_ = accD[ub_][si % 2]
                        dk_ = "accD%d_%d" % (ub_, si % 2)
                        if si == 0:
                            P.op("vector", lambda e, p=p, nq=nq, dst_=dst_: e.tensor_copy(out=dst_[:, :nq], in_=ptl[p][:, :nq]), reads=["ptl%d" % p], writes=[dk_])
                        else:
                            src_ = accD[ub_][(si - 1) % 2]
                            sk_ = "accD%d_%d" % (ub_, (si - 1) % 2)
                            P.op("vector", lambda e, p=p, nq=nq, dst_=dst_, src_=src_: e.tensor_tensor(out=dst_[:, :nq], in0=src_[:, :nq], in1=ptl[p][:, :nq], op=ALU.add),
                                 reads=["ptl%d" % p, sk_], writes=[dk_])
                if i >= LA:
                    u, (kT, v, mask), first, last = steps[i - LA]
                    nq = u["nq"]
                    p = slots.pop(i - LA)
                    ob_ = 4 + (u["uid"] % 2)
                    db_ = 6 + (u["uid"] % 2)
                    P.op("tensor", lambda e, v=v, p=p, ob_=ob_, nq=nq, first=first, last=last: e.matmul(bank[ob_][:, :nq], lhsT=v, rhs=ptl[p][:, :nq], start=first, stop=last),
                         reads=u["rk"] + ["ptl%d" % p], writes=["bank%d" % ob_])
                    if not u.get("accm"):
                        P.op("tensor", lambda e, p=p, db_=db_, nq=nq, first=first, last=last: e.matmul(bank[db_][:, :nq], lhsT=ones[:], rhs=ptl[p][:, :nq], start=first, stop=last),
                             reads=["ones", "ptl%d" % p], writes=["bank%d" % db_])
                    elif last:
                        ui_ = u["uid"] % 2
                        fi_ = (len(u["kblocks"]) - 1) % 2
                        P.op("tensor", lambda e, db_=db_, nq=nq, ui_=ui_, fi_=fi_: e.matmul(bank[db_][:, :nq], lhsT=onesf[:], rhs=accD[ui_][fi_][:, :nq], start=True, stop=True),
                             reads=["onesf", "accD%d_%d" % (ui_, fi_)], writes=["bank%d" % db_])
                    if last:
                        u["epi"](u, ob_, db_)

        def epi_plain(sink_col=None):
            def f(u, ob_, db_):
                nq = u["nq"]
                if sink_col is not None:
                    P.op("vector", lambda e: e.tensor_scalar(out=rec[:, :nq], in0=bank[db_][:, :nq], scalar1=esink[:, sink_col:sink_col + 1], scalar2=None, op0=ALU.add),
                         reads=["bank%d" % db_, "esink"], writes=["rec"])
                    P.op("vector", lambda e: e.reciprocal(out=rec[:, :nq], in_=rec[:, :nq]), reads=["rec"], writes=["rec"])
                else:
                    P.op("vector", lambda e: e.reciprocal(out=rec[:, :nq], in_=bank[db_][:, :nq]), reads=["bank%d" % db_], writes=["rec"])
                P.op("vector", lambda e: e.tensor_tensor(out=u["out"], in0=bank[ob_][:, :nq], in1=rec[:, :nq], op=ALU.mult),
                     reads=["bank%d" % ob_, "rec"], writes=["OT"])
            return f

        def epi_diff(m):
            def f(u, ob_, db_):
                nq = u["nq"]
                dst = oa if m == 0 else obb
                dk = "oa" if m == 0 else "obb"
                P.op("vector", lambda e: e.reciprocal(out=rec[:, :nq], in_=bank[db_][:, :nq]), reads=["bank%d" % db_], writes=["rec"])
                P.op("vector", lambda e: e.tensor_tensor(out=dst[:, :nq], in0=bank[ob_][:, :nq], in1=rec[:, :nq], op=ALU.mult),
                     reads=["bank%d" % ob_, "rec"], writes=[dk])
                if m == 1:
                    P.op("vector", lambda e: e.scalar_tensor_tensor(out=od[:, :nq], in0=obb[:, :nq], scalar=lsc[:, 3:4], in1=oa[:, :nq], op0=ALU.mult, op1=ALU.add),
                         reads=["oa", "obb", "neglam"], writes=["od"])
                    P.op("scalar", lambda e: e.activation(out=osq[:, :nq], in_=od[:, :nq], func=AF.Square), reads=["od"], writes=["osq"])
                    b = state["psi"] % 4
                    state["psi"] += 1
                    P.op("tensor", lambda e: e.matmul(bank[b][:, :nq], lhsT=ones[:], rhs=osq[:, :nq], start=True, stop=True), reads=["ones", "osq"], writes=["bank%d" % b])
                    P.op("scalar", lambda e: e.activation(out=rec[:, :nq], in_=bank[b][:, :nq], func=AF.Sqrt, bias=epst[:], scale=1.0 / 128), reads=["bank%d" % b, "epst"], writes=["rec"])
                    P.op("vector", lambda e: e.reciprocal(out=rec[:, :nq], in_=rec[:, :nq]), reads=["rec"], writes=["rec"])
                    P.op("vector", lambda e: e.scalar_tensor_tensor(out=u["out"], in0=od[:, :nq], scalar=subs[:, 0:1], in1=rec[:, :nq], op0=ALU.mult, op1=ALU.mult),
                         reads=["od", "subs", "rec"], writes=["OT"])
            return f

        qgroups = [(0, 512), (512, 512)] + ([(1024, 64)] if has_ctx else [])

        fence(["KT", "VV", "QQ"])
        KTv = arena[:, 0:2 * KEYS].rearrange("p (h t) -> p h t", h=2)
        VAv = arena[:, 2 * KEYS:2 * KEYS + NKB * 256].rearrange("p (k c) -> p k c", c=256)
        QAv = qar[:, 0:6 * ntq].rearrange("p (h t) -> p h t", h=6)
        for qq in range(4):
            P.op("sync", lambda e, qq=qq: e.dma_start(out=KTv[:, :, qq * TL:(qq + 1) * TL], in_=KTG0[qq * 512:qq * 512 + 256, :].rearrange("(h d) t -> d h t", d=128)), writes=["KT"], dma=True)
            P.op("sync", lambda e, qq=qq: e.dma_start(out=KTv[:, :, SEQ + qq * TC:SEQ + (qq + 1) * TC], in_=KTGc[qq * 1024:qq * 1024 + 256, :].rearrange("(h d) t -> d h t", d=128)), writes=["KT"], dma=True)
        for qq in range(4):
            for c_ in range(2):
                P.op("gpsimd", lambda e, qq=qq, c_=c_: e.dma_start(out=VAv[:, qq * 8 + 4 * c_:qq * 8 + 4 * c_ + 4, :], in_=VGs[c_][qq * 512:(qq + 1) * 512, 0:256].rearrange("(k p) c -> p k c", p=128)), writes=["VV"], dma=True)
        P.op("gpsimd", lambda e: e.dma_start(out=VAv[:, 32:34, :], in_=VGc[:, 0:256].rearrange("(k p) c -> p k c", p=128)), writes=["VV"], dma=True)
        P.op("sync", lambda e: e.dma_start(out=QAv, in_=QT[0:768, 0:ntq].rearrange("(h d) t -> d h t", d=128)), writes=["QQ"], dma=True)
        units = []
        for h in range(6):
            kv = h // 3
            for (q0, nq) in qgroups:
                kbs = range(NKB) if q0 < 1024 else range(32, 34)
                units.append(dict(kblocks=[(KTv[:, kv, kb * 128:(kb + 1) * 128], VAv[:, kb, kv * 128:(kv + 1) * 128], None) for kb in kbs],
                                  q=QAv[:, h, q0:q0 + nq], nq=nq, scale=128 ** -0.5, rk=["KT", "VV", "QQ"], epi=epi_plain(), out=OT[:, h, q0:q0 + nq]))
        run_units(units)

        fence(["KT", "KTc", "VV", "QQ"])
        KBv = arena[:, 0:4 * KEYS].rearrange("p (h t) -> p h t", h=4)
        VBv = arena[:, 4 * KEYS:4 * KEYS + NKB * 512].rearrange("p (k c) -> p k c", c=512)
        QBv = qar[:, 0:4 * ntq].rearrange("p (h t) -> p h t", h=4)
        for m in range(2):
            for qq in range(4):
                P.op("sync", lambda e, m=m, qq=qq: e.dma_start(out=KBv[64 * m:64 * m + 64, :, qq * TL:(qq + 1) * TL], in_=KTG1[qq * 512:qq * 512 + 512, :].rearrange("(h m d) t -> m d h t", m=2, d=64)[m]), writes=["KT"], dma=True)
                P.op("sync", lambda e, m=m, qq=qq: e.dma_start(out=KBv[64 * m:64 * m + 64, :, SEQ + qq * TC:SEQ + (qq + 1) * TC], in_=KTGc[qq * 1024 + 512:qq * 1024 + 1024, :].rearrange("(h m d) t -> m d h t", m=2, d=64)[m]), writes=["KT"], dma=True)
            P.op("sync", lambda e, m=m: e.dma_start(out=QBv[64 * m:64 * m + 64, :, :], in_=QT[768:1280, 0:ntq].rearrange("(h m d) t -> m d h t", m=2, d=64)[m]), writes=["QQ"], dma=True)
        for qq in range(4):
            for c_ in range(2):
                P.op("gpsimd", lambda e, qq=qq, c_=c_: e.dma_start(out=VBv[:, qq * 8 + 4 * c_:qq * 8 + 4 * c_ + 4, :], in_=VGs[c_][qq * 512:(qq + 1) * 512, 256:768].rearrange("(k p) c -> p k c", p=128)), writes=["VV"], dma=True)
        P.op("gpsimd", lambda e: e.dma_start(out=VBv[:, 32:34, :], in_=VGc[:, 256:768].rearrange("(k p) c -> p k c", p=128)), writes=["VV"], dma=True)
        units = []
        for h in range(4):
            for (q0, nq) in qgroups:
                kbs = range(NKB) if q0 < 1024 else range(32, 34)
                for m in range(2):
                    rs = slice(64 * m, 64 * m + 64)
                    units.append(dict(kblocks=[(KBv[rs, h, kb * 128:(kb + 1) * 128], VBv[:, kb, h * 128:(h + 1) * 128], None) for kb in kbs],
                                      q=QBv[rs, h, q0:q0 + nq], nq=nq, scale=64 ** -0.5, rk=["KT", "VV", "QQ"], epi=epi_diff(m), out=OT[:, 6 + h, q0:q0 + nq]))
        run_units(units)

        fence(["KT", "KTc", "VV", "QQ"])
        KCv = arena[:, 0:2 * 1536].rearrange("p (h t) -> p h t", h=2)
        Wt = arena[:, 3072:3072 + 5120].rearrange("p (k c) -> p k c", c=512)
        VCc = arena[:, 8192:8192 + 512].rearrange("p (k c) -> p k c", c=256)
        QCv = qar[:, 0:6 * ntq].rearrange("p (h t) -> p h t", h=6)
        for blk in range(10):
            P.op("gpsimd", lambda e, blk=blk: e.indirect_dma_start(out=Wt[:, blk, :], out_offset=None, in_=WG,
                                                                  in_offset=bass.IndirectOffsetOnAxis(ap=widx[:, blk:blk + 1], axis=0)),
                 reads=["widx"], writes=["VV"], dma=True)
        for qq in range(4):
            P.op("sync", lambda e, qq=qq: e.dma_start(out=KCv[:, :, 1280 + qq * TC:1280 + (qq + 1) * TC], in_=KTGc[qq * 1024 + 256:qq * 1024 + 512, :].rearrange("(h d) t -> d h t", d=128)), writes=["KTc"], dma=True)
        P.op("sync", lambda e: e.dma_start(out=VCc, in_=VGc[:, 768:1024].rearrange("(k p) c -> p k c", p=128)), writes=["VV"], dma=True)
        P.op("sync", lambda e: e.dma_start(out=QCv, in_=QT[1280:2048, 0:ntq].rearrange("(h d) t -> d h t", d=128)), writes=["QQ"], dma=True)
        for rnd in range(5):
            for c in range(4):
                i_ = rnd * 4 + c
                kv, blk = i_ // 10, i_ % 10
                P.op("tensor", lambda e, c=c, kv=kv, blk=blk: e.transpose(out=pTb[:, c, :], in_=Wt[:, blk, 256 + kv * 128:256 + (kv + 1) * 128], identity=idt[:]),
                     reads=["VV", "idt"], writes=["bank7"])
            for c in range(4):
                i_ = rnd * 4 + c
                kv, blk = i_ // 10, i_ % 10
                P.op("scalar", lambda e, c=c, kv=kv, blk=blk: e.copy(out=KCv[:, kv, blk * 128:(blk + 1) * 128], in_=pTb[:, c, :]), reads=["bank7"], writes=["KT"])
        units = []
        for h in range(6):
            kv = h // 3
            ctxk = [(KCv[:, kv, kb * 128:(kb + 1) * 128], VCc[:, kb - 10, kv * 128:(kv + 1) * 128], None) for kb in (10, 11)]
            for n in range(8):
                kbl = []
                for w_ in range(3):
                    kb = n + w_
                    mask = None if w_ == 1 else bm[:, n, 0 if w_ == 0 else 1, :]
                    kbl.append((KCv[:, kv, kb * 128:(kb + 1) * 128], Wt[:, kb, kv * 128:(kv + 1) * 128], mask))
                kbl = kbl + ctxk
                units.append(dict(kblocks=kbl, q=QCv[:, h, n * 128:(n + 1) * 128], nq=128, scale=128 ** -0.5, rk=["KT", "KTc", "VV", "QQ"],
                                  epi=epi_plain(h), out=OT[:, 10 + h, n * 128:(n + 1) * 128]))
            if has_ctx:
                units.append(dict(kblocks=list(ctxk), q=QCv[:, h, 1024:1088], nq=64, scale=128 ** -0.5, rk=["KT", "KTc", "VV", "QQ"],
                                  epi=epi_plain(h), out=OT[:, 10 + h, 1024:1088]))
        run_units(units)

        fence(["wsl0", "wsl1", "wsl0b", "wsl1b", "wsl0c", "wsl1c"] + ["mT%d_%d" % (a_, b_) for a_ in range(4) for b_ in range(ntiles)])
        mT = arena[:, 0:16 * ntq].rearrange("p (k t) -> p k t", k=16)
        wsl = [arena[:, 16 * ntq + s * 8192:16 * ntq + (s + 1) * 8192].rearrange("p (k c) -> p k c", c=512) for s in range(2)]
        wi = 0
        gi = 0

        def load_wbr(i_):
            s_ = i_ % 2
            wk_ = "wsl%d" % s_
            if i_ < 4:
                cs_ = slice(i_ * 512, (i_ + 1) * 512)
                P.op("gpsimd", lambda e: e.dma_start(out=wsl[s_][:, 0:6, :], in_=w_br_a[:, cs_].rearrange("(c p) n -> p c n", p=128)), writes=[wk_, wk_ + "b", wk_ + "c"], dma=True)
                P.op("gpsimd", lambda e: e.dma_start(out=wsl[s_][:, 6:10, :], in_=w_br_b[:, cs_].rearrange("(c p) n -> p c n", p=128)), reads=[wk_], writes=[wk_ + "b"], dma=True)
                P.op("gpsimd", lambda e: e.dma_start(out=wsl[s_][:, 10:16, :], in_=w_br_c[:, cs_].rearrange("(c p) n -> p c n", p=128)), reads=[wk_], writes=[wk_ + "c"], dma=True)
            elif i_ < 8:
                cs_ = slice((i_ - 4) * 512, (i_ - 3) * 512)
                P.op("gpsimd", lambda e: e.dma_start(out=wsl[s_][:], in_=w_out[:, cs_].rearrange("(k p) n -> p k n", p=128)), writes=[wk_, wk_ + "b", wk_ + "c"], dma=True)

        load_wbr(0)
        d1pend = []
        for dg in range(4):
            s = wi % 2
            wi += 1
            wk = "wsl%d" % s
            cs = slice(dg * 512, (dg + 1) * 512)
            load_wbr(wi)
            for t in range(ntiles):
                r = rows(t)
                r0 = t * 128
                g_ = gi % 2
                gi += 1
                gk = "gsl%d" % g_
                P.op("sync", lambda e, g_=g_, r=r, r0=r0, cs=cs: e.dma_start(out=gsl[g_][:r, :, :], in_=Gin[r0:r0 + r, :].rearrange("t (b c) -> t b c", b=3)[:, :, cs]), writes=[gk], dma=True)
                bo = 3 * (g_ % 2)
                for (bi, c0, c1) in ((bo, 0, 6), (bo + 1, 6, 10), (bo + 2, 10, 16)):
                    for c in range(c0, c1):
                        P.op("tensor", lambda e, bi=bi, c=c, c0=c0, c1=c1, s=s, r=r, r0=r0: e.matmul(bank[bi][:r, :], lhsT=OT[:, c, r0:r0 + r], rhs=wsl[s][:, c, :], start=(c == c0), stop=(c == c1 - 1)),
                             reads=["OT", wk, wk + "b", wk + "c"], writes=["bank%d" % bi])
                mk = "mtmp%d" % g_
                P.op("vector", lambda e, g_=g_, r=r, bo=bo: e.tensor_tensor(out=mtmp[g_][:r, :], in0=bank[bo][:r, :], in1=gsl[g_][:r, 0, :], op=ALU.mult), reads=["bank%d" % bo, gk], writes=[mk])
                P.op("vector", lambda e, g_=g_, r=r, bo=bo: e.tensor_tensor(out=xsl[g_][:r, :], in0=bank[bo + 1][:r, :], in1=gsl[g_][:r, 1, :], op=ALU.mult), reads=["bank%d" % (bo + 1), gk], writes=["xsl%d" % g_])
                P.op("vector", lambda e, g_=g_, r=r: e.tensor_tensor(out=mtmp[g_][:r, :], in0=mtmp[g_][:r, :], in1=xsl[g_][:r, :], op=ALU.add), reads=[mk, "xsl%d" % g_], writes=[mk])
                P.op("vector", lambda e, g_=g_, r=r, bo=bo: e.tensor_tensor(out=xsl[g_][:r, :], in0=bank[bo + 2][:r, :], in1=gsl[g_][:r, 2, :], op=ALU.mult), reads=["bank%d" % (bo + 2), gk], writes=["xsl%d" % g_])
                P.op("vector", lambda e, g_=g_, r=r: e.tensor_tensor(out=mb[g_][:r, :], in0=mtmp[g_][:r, :], in1=xsl[g_][:r, :], op=ALU.add), reads=[mk, "xsl%d" % g_], writes=["mb%d" % g_])
                def d1tail(g_=g_, r=r, r0=r0, dg=dg, t=t):
                    for c in range(4):
                        P.op("tensor", lambda e, c=c: e.transpose(out=pTb[:, c, :r], in_=mb[g_][:r, c * 128:(c + 1) * 128], identity=idt[:r, :r]),
                             reads=["mb%d" % g_, "idt"], writes=["bank7"])
                    P.op("scalar", lambda e: e.copy(out=mT[:, dg * 4:(dg + 1) * 4, r0:r0 + r], in_=pTb[:, :, :r]), reads=["bank7"], writes=["mT%d_%d" % (dg, t)])

                d1pend.append(d1tail)
                while len(d1pend) > 1:
                    d1pend.pop(0)()
        for f_ in d1pend:
            f_()

        ui = 0
        for dg in range(4):
            s = wi % 2
            wi += 1
            wk = "wsl%d" % s
            cs = slice(dg * 512, (dg + 1) * 512)
            load_wbr(wi)
            for t in range(ntiles):
                r = rows(t)
                r0 = t * 128
                b = ui % 6
                g_ = ui % 2
                ui += 1
                bk = "bank%d" % b
                P.op("sync", lambda e, g_=g_, r=r, r0=r0, cs=cs: e.dma_start(out=xsl[g_][:r, :], in_=xin[r0:r0 + r, cs]), writes=["xsl%d" % g_], dma=True)
                for k in range(16):
                    P.op("tensor", lambda e, k=k, b=b, s=s, r=r, r0=r0: e.matmul(bank[b][:r, :], lhsT=mT[:, k, r0:r0 + r], rhs=wsl[s][:, k, :], start=(k == 0), stop=(k == 15)),
                         reads=["mT%d_%d" % (k // 4, t), wk], writes=[bk])
                gw = 0 if t < 8 else 1
                P.op("vector", lambda e, b=b, g_=g_, r=r, cs=cs, gw=gw: e.tensor_tensor(out=mtmp[g_][:r, :], in0=bank[b][:r, :], in1=g1t[gw][:r, cs], op=ALU.mult),
                     reads=[bk, "g1t%d" % gw], writes=["mtmp%d" % g_])
                P.op("vector", lambda e, g_=g_, r=r: e.tensor_tensor(out=mtmp[g_][:r, :], in0=mtmp[g_][:r, :], in1=xsl[g_][:r, :], op=ALU.add),
                     reads=["mtmp%d" % g_, "xsl%d" % g_], writes=["mtmp%d" % g_])
                P.op("sync", lambda e, g_=g_, r=r, r0=r0, cs=cs: e.dma_start(out=x1o[r0:r0 + r, cs], in_=mtmp[g_][:r, :]), reads=["mtmp%d" % g_], writes=["x1d%d" % t], dma=True)

        fence(["x1t0", "x1t1", "h2f0", "h2f1", "h2b0", "h2b1", "h2T0", "h2T1", "h2T2", "h2T3", "am2", "bs2"])
        f0_ = abase // 4
        f32v = S.v[F32][:, f0_:f0_ + ARENA // 2]
        x1t = [f32v[:, 0:2048], f32v[:, 12288:14336]]
        h2f = [f32v[:, 2048:4096], f32v[:, 14336:16384]]
        am2 = f32v[:, 4096:6144]
        bs2 = f32v[:, 6144:8192]
        h2T = f32v[:, 8192:10240].rearrange("p (k t) -> p k t", k=16)
        h2b = [arena[:, 20480:22528], arena[:, 22528:24576]]

        def load_mod2(which):
            P.op("sync", lambda e: e.dma_start(out=am2, in_=modv(T, l, which, 4).partition_broadcast(128)), writes=["am2"], dma=True)
            P.op("sync", lambda e: e.dma_start(out=bs2, in_=normf.partition_broadcast(128)), writes=["bs2"], dma=True)
            P.op("vector", lambda e: e.scalar_tensor_tensor(out=am2, in0=am2, scalar=1.0, in1=bs2, op0=ALU.add, op1=ALU.mult), reads=["am2", "bs2"], writes=["am2"])
            P.op("sync", lambda e: e.dma_start(out=bs2, in_=modv(T, l, which, 3).partition_broadcast(128)), reads=["bs2"], writes=["bs2"], dma=True)

        load_mod2(0)

        def stage1(t):
            r = rows(t)
            r0 = t * 128
            u_ = t % 2
            xk_, hk_, bk_ = "x1t%d" % u_, "h2f%d" % u_, "h2b%d" % u_
            if t == 8:
                load_mod2(1)
            P.op("sync", lambda e: e.dma_start(out=x1t[u_][:r, :], in_=x1o[r0:r0 + r, :]), reads=["x1d%d" % t], writes=[xk_], dma=True)
            P.op("scalar", lambda e: e.activation(out=h2f[u_][:r, :], in_=x1t[u_][:r, :], func=AF.Square, accum_out=sm[:r, 0:1]), reads=[xk_], writes=[hk_, "sm0"])
            P.op("scalar", lambda e: e.activation(out=sm[:r, 1:2], in_=sm[:r, 0:1], func=AF.Sqrt, bias=epst[:r, :], scale=1.0 / D), reads=["sm0", "epst"], writes=["sm1"])
            P.op("vector", lambda e: e.reciprocal(out=sm[:r, 1:2], in_=sm[:r, 1:2]), reads=["sm1"], writes=["sm1"])
            P.op("vector", lambda e: e.scalar_tensor_tensor(out=h2f[u_][:r, :], in0=x1t[u_][:r, :], scalar=sm[:r, 1:2], in1=am2[:r, :], op0=ALU.mult, op1=ALU.mult),
                 reads=[xk_, "sm1", "am2"], writes=[hk_])
            P.op("vector", lambda e: e.tensor_tensor(out=h2f[u_][:r, :], in0=h2f[u_][:r, :], in1=bs2[:r, :], op=ALU.add), reads=[hk_, "bs2"], writes=[hk_])
            P.op("scalar", lambda e: e.copy(out=h2b[u_][:r, :], in_=h2f[u_][:r, :]), reads=[hk_], writes=[bk_])
            h2dst = L["h2l%d" % (t // 2)][(t % 2) * 128:(t % 2) * 128 + r, :] if t < 8 else L["h2ctx"][0:r, :]
            P.op("sync", lambda e: e.dma_start(out=h2dst, in_=h2b[u_][:r, :]), reads=[bk_], dma=True)

        def stage2(t):
            r = rows(t)
            r0 = t * 128
            u_ = t % 2
            hk_ = "h2f%d" % u_
            for q4 in range(4):
                for c in range(4):
                    k = q4 * 4 + c
                    P.op("tensor", lambda e, q4=q4, c=c, k=k: e.transpose(out=bank[q4][:, c * 128:c * 128 + r], in_=h2f[u_][:r, k * 128:(k + 1) * 128], identity=idf[:r, :r]),
                         reads=[hk_, "idf"], writes=["bank%d" % q4])
                P.op("vector" if q4 % 2 == 0 else "scalar",
                     (lambda e, q4=q4: e.tensor_copy(out=h2T[:, q4 * 4:(q4 + 1) * 4, :r], in_=bank[q4][:].rearrange("p (c t) -> p c t", c=4)[:, :, :r])) if q4 % 2 == 0 else
                     (lambda e, q4=q4: e.copy(out=h2T[:, q4 * 4:(q4 + 1) * 4, :r], in_=bank[q4][:].rearrange("p (c t) -> p c t", c=4)[:, :, :r])),
                     reads=["bank%d" % q4], writes=["h2T%d" % q4])
            for k in range(16):
                P.op("tensor", lambda e, k=k: e.matmul(bank[4][:r, :NEXP], lhsT=h2T[:, k, :r], rhs=wr[:, k, :], start=(k == 0), stop=(k == 15)),
                     reads=["h2T%d" % (k // 4), "wr"], writes=["bank4"])
            a_ = t % 2
            P.op("vector", lambda e: e.tensor_reduce(out=sm[:r, 2:3], in_=bank[4][:r, :NEXP], axis=AX.X, op=ALU.max), reads=["bank4"], writes=["sm2"])
            P.op("vector", lambda e: e.tensor_scalar(out=sm[:r, 2:3], in0=sm[:r, 2:3], scalar1=-1.0, scalar2=None, op0=ALU.mult), reads=["sm2"], writes=["sm2"])
            P.op("scalar", lambda e: e.activation(out=afft[a_][:r, :], in_=bank[4][:r, :NEXP], func=AF.Exp, bias=sm[:r, 2:3], accum_out=sm[:r, 3:4]),
                 reads=["bank4", "sm2"], writes=["afft%d" % a_, "sm3"])
            P.op("vector", lambda e: e.reciprocal(out=sm[:r, 3:4], in_=sm[:r, 3:4]), reads=["sm3"], writes=["sm3"])
            P.op("vector", lambda e: e.tensor_scalar(out=afft[a_][:r, :], in0=afft[a_][:r, :], scalar1=sm[:r, 3:4], scalar2=None, op0=ALU.mult),
                 reads=["afft%d" % a_, "sm3"], writes=["afft%d" % a_])
            P.op("tensor", lambda e: e.transpose(out=bank[5][:NEXP, :r], in_=afft[a_][:r, :], identity=idf[:r, :r]), reads=["afft%d" % a_, "idf"], writes=["bank5"])
            P.op("scalar", lambda e: e.copy(out=affTs[a_][:, :r], in_=bank[5][:NEXP, :r]), reads=["bank5"], writes=["affTs%d" % a_])
            adst = L["affTl"][:, r0:r0 + r] if t < 8 else L["affTc"][:, 0:r]
            P.op("sync", lambda e: e.dma_start(out=adst, in_=affTs[a_][:, :r]), reads=["affTs%d" % a_], dma=True)

        stage1(0)
        for t in range(ntiles):
            if t + 1 < ntiles:
                stage1(t + 1)
            stage2(t)
    S.P.barrier()
    if not CC_OVERLAP:
        for c_ in range(4):
            _cc(P, "AllGather", ALU.bypass, L["h2l%d" % c_], L["h2G"][c_ * 1024:(c_ + 1) * 1024, :], [], ["h2G%d" % c_])
    pairs = [("affTl", "affG")] + ([("h2ctx", "h2cG"), ("affTc", "affcG")] if has_ctx else [])
    for (a, b) in pairs:
        _cc(P, "AllGather", ALU.bypass, L[a], L[b], [], [b])


NEL = 4


def phase_C(S, T, l, has_ctx):
    P = S.P
    S.phase()
    L = T["L"][l]
    nt = 5 if has_ctx else 4
    ntok = CAP_L + (CAP_C if has_ctx else 0)
    wg, wu, wd = T["wg"][l], T["wu"][l], T["wd"][l]
    h2G, acc = L["h2G"], L["acc_lat"]
    idx_d, gv_d, alat = L["idx_d"], L["gv_d"], L["alat"]

    def rows(t):
        return 128 if t < 4 else 32

    def row0(t):
        return t * 128

    C = S
    ag = C.sb([16, TL], F32)
    arow = C.sb([16, 1], U32)
    at = C.sb([4, SEQ], F32)
    vals = C.sb([4, CAP_L], F32)
    idx = C.sb([4, CAP_L], U32)
    if has_ctx:
        agc = C.sb([16, TC], F32)
        atc = C.sb([4, CTX], F32)
        valsc = C.sb([4, CAP_C], F32)
        idxc = C.sb([4, CAP_C], U32)
        gTc = C.sb([32, 4], F32)
        idxTc = C.sb([32, 4], U32)
    gT = C.sb([128, 16], F32)
    idxT = C.sb([128, 16], U32)
    zt = C.sb([128, D], F32)
    fsc = C.sb([128, 1], F32)
    idt = C.sb([128, 128], BF16)
    xg = [C.sb([128, D], BF16) for _ in range(2)]
    xgT = C.sb([128, 16, ntok], BF16)
    actT = C.sb([128, 8, ntok], BF16)
    wgs = [C.sb([128, 16, 512], BF16) for _ in range(2)]
    wus = [C.sb([128, 16, 512], BF16) for _ in range(2)]
    wds = C.sb([128, 8, D], BF16)
    su = [C.sb([128, 512], F32) for _ in range(2)]
    yt = [C.sb([128, D], F32) for _ in range(2)]
    pT = C.ps([128, 16, 128], BF16)
    pu = [C.ps([128, 512], F32) for _ in range(2)]
    pv = [C.ps([128, 512], F32) for _ in range(2)]
    py = [C.ps([128, 512], F32) for _ in range(2)]

    P.op("sync", lambda e: e.dma_start(out=idt[:], in_=T["ident"]), writes=["idt"], dma=True)
    P.op("sync", lambda e: e.dma_start(out=arow[:], in_=T["arow"]), writes=["arow"], dma=True)
    PRE = None
    def load_slab(el_, slab_):
        if el_ >= NEL:
            return
        cs_ = slice(slab_ * 512, (slab_ + 1) * 512)
        P.op("gpsimd", lambda e: e.dma_start(out=wgs[slab_][:], in_=wg[el_][:, cs_].rearrange("(k p) f -> p k f", p=128)), writes=["wgs%d" % slab_], dma=True)
        P.op("gpsimd", lambda e: e.dma_start(out=wus[slab_][:], in_=wu[el_][:, cs_].rearrange("(k p) f -> p k f", p=128)), writes=["wus%d" % slab_], dma=True)

    def load_wd(el_):
        if el_ >= NEL:
            return
        P.op("gpsimd", lambda e: e.dma_start(out=wds[:], in_=wd[el_].rearrange("(f p) d -> p f d", p=128)), writes=["wds"], dma=True)

    load_wd(0)
    load_slab(0, 0)
    load_slab(0, 1)
    P.op("gpsimd", lambda e: e.indirect_dma_start(out=ag[:, :], out_offset=None, in_=L["affG"], in_offset=bass.IndirectOffsetOnAxis(ap=arow[:, 0:1], axis=0)),
         reads=["arow"], writes=["ag"], dma=True)
    P.op("sync", lambda e: e.dma_start(out=alat, in_=ag[:]), reads=["ag"], writes=["alat"], dma=True)
    for c_ in range(4):
        P.op("sync", lambda e, c_=c_: e.dma_start(out=at[:, c_ * 1024:(c_ + 1) * 1024].rearrange("e (q t) -> e q t", q=4), in_=alat.rearrange("(e q) (c t) -> e c q t", q=4, c=4)[:, c_]),
             reads=["alat"], writes=["at%d" % c_], dma=True)
    if has_ctx:
        P.op("gpsimd", lambda e: e.indirect_dma_start(out=agc[:, :], out_offset=None, in_=L["affcG"], in_offset=bass.IndirectOffsetOnAxis(ap=arow[:, 0:1], axis=0)),
             reads=["arow"], writes=["agc"], dma=True)
        P.op("sync", lambda e: e.dma_start(out=L["actx"], in_=agc[:]), reads=["agc"], writes=["actx"], dma=True)
        P.op("sync", lambda e: e.dma_start(out=atc[:], in_=L["actx"].rearrange("(e q) t -> e (q t)", q=4)), reads=["actx"], writes=["atc"], dma=True)
    P.op("gpsimd", lambda e: e.memset(zt[:], 0.0), writes=["zt"])
    zkeys = []
    zi = 0
    for b0 in range(0, SEQ // 128, 4):
        zk = "z%d" % zi
        zkeys.append(zk)
        P.op("sync" if zi % 2 == 0 else "gpsimd",
             lambda e, b0=b0: e.dma_start(out=acc[b0 * 128:(b0 + 4) * 128, :].rearrange("(n p) d -> p n d", p=128), in_=zt[:].unsqueeze(1).to_broadcast([128, 4, D])),
             reads=["zt"], writes=[zk], dma=True)
        zi += 1
    if has_ctx:
        zkeys.append("zc")
        P.op("sync", lambda e: e.dma_start(out=L["acc_ctx"].rearrange("(n p) d -> p n d", p=128), in_=zt[:].unsqueeze(1).to_broadcast([128, 2, D])), reads=["zt"], writes=["zc"], dma=True)
    gkeys = ["accg0", "accg1"]
    P.op("gpsimd", lambda e: e.memset(fsc[:], 0.0), reads=zkeys, writes=gkeys + ["fsc"])

    def topk(src, v_, i_, k, nm):
        for it in range(k // 8):
            sl = slice(it * 8, (it + 1) * 8)
            P.op("vector", lambda e, sl=sl: e.max(out=v_[:, sl], in_=src[:]), reads=[nm], writes=[nm + "v"])
            P.op("vector", lambda e, sl=sl: e.max_index(out=i_[:, sl], in_max=v_[:, sl], in_values=src[:]), reads=[nm, nm + "v"], writes=[nm + "i"])
            if it < k // 8 - 1:
                P.op("vector", lambda e, sl=sl: e.match_replace(out=src[:], in_to_replace=v_[:, sl], in_values=src[:], imm_value=-1.0), reads=[nm + "v", nm], writes=[nm])

    P.op("vector", lambda e: e.memset(fsc[0:4, :], 0.0), reads=["at0", "at1", "at2", "at3"], writes=["at", "fsc"])
    topk(at, vals, idx, CAP_L, "at")
    if has_ctx:
        topk(atc, valsc, idxc, CAP_C, "atc")
    P.op("sync", lambda e: e.dma_start(out=idx_d, in_=idx[:]), reads=["ati"], writes=["idx_d"], dma=True)
    P.op("sync", lambda e: e.dma_start(out=gv_d, in_=vals[:]), reads=["atv"], writes=["gv_d"], dma=True)
    P.op("sync", lambda e: e.dma_start(out=idxT[:].rearrange("p (r j) -> p r j", r=4), in_=idx_d.rearrange("r (j p) -> p r j", p=128), allow_slow_non_contiguous=True),
         reads=["idx_d"], writes=["idxT"], dma=True)
    P.op("sync", lambda e: e.dma_start(out=gT[:].rearrange("p (r j) -> p r j", r=4), in_=gv_d.rearrange("r (j p) -> p r j", p=128), allow_slow_non_contiguous=True),
         reads=["gv_d"], writes=["gT"], dma=True)
    if has_ctx:
        P.op("sync", lambda e: e.dma_start(out=L["idxc_d"], in_=idxc[:]), reads=["atci"], writes=["idxc_d"], dma=True)
        P.op("sync", lambda e: e.dma_start(out=L["gvc_d"], in_=valsc[:]), reads=["atcv"], writes=["gvc_d"], dma=True)
        P.op("sync", lambda e: e.dma_start(out=idxTc[:], in_=L["idxc_d"].rearrange("r c -> c r"), allow_slow_non_contiguous=True),
             reads=["idxc_d"], writes=["idxT"], dma=True)
        P.op("sync", lambda e: e.dma_start(out=gTc[:], in_=L["gvc_d"].rearrange("r c -> c r"), allow_slow_non_contiguous=True),
             reads=["gvc_d"], writes=["gT"], dma=True)

    cgroups = [(0, 512)] + ([(512, 32)] if has_ctx else [])

    xi = 0
    wi = 0
    ui = 0
    yi = 0
    for el in range(NEL):
        for t in range(nt):
            r = rows(t)
            r0 = row0(t)
            x_ = xi % 2
            xi += 1
            xk = "xg%d" % x_
            if t < 4:
                col = el * 4 + t
                P.op("gpsimd", lambda e, x_=x_, col=col: e.indirect_dma_start(out=xg[x_][:, :], out_offset=None, in_=h2G,
                                                                             in_offset=bass.IndirectOffsetOnAxis(ap=idxT[:, col:col + 1], axis=0)),
                     reads=["idxT"], writes=[xk], dma=True)
            else:
                P.op("gpsimd", lambda e, x_=x_, el=el: e.indirect_dma_start(out=xg[x_][0:32, :], out_offset=None, in_=L["h2cG"],
                                                                           in_offset=bass.IndirectOffsetOnAxis(ap=idxTc[:, el:el + 1], axis=0)),
                     reads=["idxT"], writes=[xk], dma=True)
            for k in range(16):
                P.op("tensor", lambda e, k=k, x_=x_, r=r: e.transpose(out=pT[:, k, :r], in_=xg[x_][:r, k * 128:(k + 1) * 128], identity=idt[:r, :r]),
                     reads=[xk, "idt"], writes=["pT"])
            P.op("scalar", lambda e, r=r, r0=r0: e.copy(out=xgT[:, :, r0:r0 + r], in_=pT[:, :, :r]), reads=["pT"], writes=["xgT%d" % t])
        for slab in range(2):
            w_ = slab
            cs = slice(slab * 512, (slab + 1) * 512)
            for fbl in range(4):
                fb = slab * 4 + fbl
                fs = slice(fbl * 128, (fbl + 1) * 128)
                for (c0, n) in cgroups:
                    u_ = ui % 2
                    ui += 1
                    tk = ["xgT%d" % t for t in (range(4) if c0 == 0 else (4,))]
                    for k in range(16):
                        P.op("tensor", lambda e, k=k, u_=u_, w_=w_, fs=fs, c0=c0, n=n: e.matmul(pu[u_][:, :n], lhsT=wgs[w_][:, k, fs], rhs=xgT[:, k, c0:c0 + n], start=(k == 0), stop=(k == 15)),
                             reads=tk + ["wgs%d" % w_], writes=["pu%d" % u_])
                    for k in range(16):
                        P.op("tensor", lambda e, k=k, u_=u_, w_=w_, fs=fs, c0=c0, n=n: e.matmul(pv[u_][:, :n], lhsT=wus[w_][:, k, fs], rhs=xgT[:, k, c0:c0 + n], start=(k == 0), stop=(k == 15)),
                             reads=tk + ["wus%d" % w_], writes=["pv%d" % u_])
                    P.op("scalar", lambda e, u_=u_, n=n: e.activation(out=su[u_][:, :n], in_=pu[u_][:, :n], func=AF.Silu), reads=["pu%d" % u_], writes=["su%d" % u_])
                    P.op("vector", lambda e, u_=u_, fb=fb, c0=c0, n=n: e.tensor_tensor(out=actT[:, fb, c0:c0 + n], in0=su[u_][:, :n], in1=pv[u_][:, :n], op=ALU.mult),
                         reads=["su%d" % u_, "pv%d" % u_], writes=["actT%d_%d" % (fb, c0)])
                    if fbl == 3 and (c0, n) == cgroups[-1]:
                        load_slab(el + 1, slab)
        for t in range(nt):
            r = rows(t)
            r0 = row0(t)
            y_ = yi % 2
            yi += 1
            yk = "yt%d" % y_
            cg0 = 0 if t < 4 else 512
            ak = ["actT%d_%d" % (f, cg0) for f in range(8)]
            if t < 4:
                col = el * 4 + t
                gsc = gT[:, col:col + 1]
            else:
                gsc = gTc[:, el:el + 1]
            for dg in range(4):
                b_ = (t * 4 + dg) % 2
                for f in range(8):
                    P.op("tensor", lambda e, f=f, b_=b_, dg=dg, r=r, r0=r0: e.matmul(py[b_][:r, :], lhsT=actT[:, f, r0:r0 + r], rhs=wds[:, f, dg * 512:(dg + 1) * 512], start=(f == 0), stop=(f == 7)),
                         reads=ak + ["wds"], writes=["py%d" % b_])
                P.op("vector", lambda e, b_=b_, dg=dg, r=r, y_=y_, gsc=gsc: e.tensor_scalar(out=yt[y_][:r, dg * 512:(dg + 1) * 512], in0=py[b_][:r, :], scalar1=gsc[:r, :], scalar2=None, op0=ALU.mult),
                     reads=["py%d" % b_, "gT"], writes=[yk + "_%d" % dg])
            ykeys = [yk + "_%d" % dg for dg in range(4)]
            if t < 4:
                P.op("gpsimd", lambda e, y_=y_, col=col: e.indirect_dma_start(out=acc, out_offset=bass.IndirectOffsetOnAxis(ap=idxT[:, col:col + 1], axis=0),
                                                                             in_=yt[y_][:, :], in_offset=None, compute_op=ALU.add),
                     reads=ykeys + ["idxT"], writes=["accg0"], dma=True)
            else:
                P.op("gpsimd", lambda e, y_=y_, el=el: e.indirect_dma_start(out=L["acc_ctx"], out_offset=bass.IndirectOffsetOnAxis(ap=idxTc[:, el:el + 1], axis=0),
                                                                           in_=yt[y_][0:32, :], in_offset=None, compute_op=ALU.add),
                     reads=ykeys + ["idxT"], writes=["accg1"], dma=True)
            if t == nt - 1:
                load_wd(el + 1)
    S.P.barrier()
    for c_ in range(4):
        _cc(P, "ReduceScatter", ALU.add, L["acc_lat"][c_ * 1024:(c_ + 1) * 1024, :], L["comb%d" % c_], [], ["comb%d" % c_])
    if has_ctx:
        _cc(P, "ReduceScatter", ALU.add, L["acc_ctx"], L["comb_ctx"], [], ["comb_ctx"])


def build_fused():
    nc = bass.Bass("TRN2", target_bir_lowering=False)
    T = {}
    ein = lambda name, shape, dt: T.__setitem__(name, _din(nc, name, shape, dt))
    ein("xin", [TT, D], F32)
    ein("cT", [128, 16, 2], F32)
    ein("wm", [D, MW], F32)
    ein("bm", [MW], F32)
    ein("norm_mix", [2, D], F32)
    ein("norm_ffn", [2, D], F32)
    ein("w_in", [2, D, IN_W], F32)
    ein("g128", [2, 4, 128], F32)
    ein("g64", [2, 2, 64], F32)
    ein("cos128", [TL, 128], F32)
    ein("sin128", [TL, 128], F32)
    ein("cos64", [TL, 64], F32)
    ein("sin64", [TL, 64], F32)
    ein("ident", [128, 128], BF16)
    ein("identf", [128, 128], F32)
    ein("bandm", [128, 8, 2, 128], BF16)
    ein("widx", [128, 10], U32)
    ein("arow", [16, 1], U32)
    ein("w_br_a", [2, 768, D], F32)
    ein("w_br_b", [2, 512, D], F32)
    ein("w_br_c", [2, 768, D], F32)
    ein("w_out", [2, D, D], F32)
    ein("w_router", [2, D, NEXP], F32)
    ein("lamv", [2, 4, 64], F32)
    ein("subln", [2, 128], F32)
    ein("sink", [2, 6], F32)
    ein("wg", [2, NEL, D, FF], F32)
    ein("wu", [2, NEL, D, FF], F32)
    ein("wd", [2, NEL, FF, D], F32)
    out = _dout(nc, "out", [TL, D], F32)
    with contextlib.ExitStack() as es:
        S = State(nc, es)
        T["modloc"] = S.dram([2, MW], F32)
        T["modG"] = S.dram([8, MW], F32)
        T["L"] = []
        for l in range(2):
            L = {}
            for (nm, shape, dt) in (("KTloc0", [512, TL], BF16), ("KTG0", [2048, TL], BF16), ("KTloc1", [512, TL], BF16), ("KTG1", [2048, TL], BF16),
                                    ("KTlocC", [1024, TC], BF16), ("KTGc", [4096, TC], BF16),
                                    ("Vloc0", [512, 1024], BF16), ("VG0", [2048, 1024], BF16), ("Vloc1", [512, 1024], BF16), ("VG1", [2048, 1024], BF16),
                                    ("VlocC", [TC, 1024], BF16), ("VGc", [CTX, 1024], BF16),
                                    ("WKVloc", [TL, 512], BF16), ("WG", [SEQ, 512], BF16), ("QT", [2048, TT], BF16), ("G", [TT, 6144], BF16),
                                    ("x1", [TT, D], F32), ("xcomb", [TT, D], F32),
                                    ("h2l0", [256, D], BF16), ("h2l1", [256, D], BF16), ("h2l2", [256, D], BF16), ("h2l3", [256, D], BF16), ("h2G", [SEQ, D], BF16), ("h2ctx", [TC, D], BF16), ("h2cG", [CTX, D], BF16),
                                    ("affTl", [NEXP, TL], F32), ("affG", [4 * NEXP, TL], F32), ("affTc", [NEXP, TC], F32), ("affcG", [4 * NEXP, TC], F32),
                                    ("alat", [16, TL], F32), ("actx", [16, TC], F32),
                                    ("idx_d", [4, CAP_L], U32), ("gv_d", [4, CAP_L], F32), ("idxc_d", [4, CAP_C], U32), ("gvc_d", [4, CAP_C], F32),
                                    ("acc_lat", [SEQ, D], F32), ("comb0", [256, D], F32), ("comb1", [256, D], F32), ("comb2", [256, D], F32), ("comb3", [256, D], F32), ("acc_ctx", [CTX, D], F32), ("comb_ctx", [TC, D], F32)):
                L[nm] = S.dram(shape, dt, "%s_%d" % (nm, l))
            T["L"].append(L)
        phase_M(S, T)
        phase_A(S, T, 0, False, True, 9, T["xin"], None)
        phase_B(S, T, 0, 9, T["xin"])
        phase_C(S, T, 0, True)
        phase_A(S, T, 1, True, True, 9, T["L"][0]["x1"], T["L"][1]["xcomb"])
        phase_B(S, T, 1, 8, T["L"][1]["xcomb"])
        phase_C(S, T, 1, False)
        phase_A(S, T, 2, True, False, 8, T["L"][1]["x1"], out, final_out=True)
        S.P.emit()
    return nc


_IDENT_B = np.eye(128).astype(NPBF)
_IDENT_F = np.eye(128, dtype=np.float32)
_NC = {}


def _rope_tables(dh):
    d_ax = dh // 2
    inv = (np.float32(10000.0) ** (-np.arange(0, d_ax, 2, dtype=np.float32) / np.float32(d_ax))).astype(np.float32)
    t = np.arange(SEQ)
    row = (t // 64).astype(np.float32)[:, None]
    col = (t % 64).astype(np.float32)[:, None]
    fr = row * inv
    fc = col * inv
    ang = np.concatenate([fr, fr, fc, fc], axis=-1).astype(np.float32)
    cos = np.cos(ang).astype(np.float32)
    sin = np.sin(ang).astype(np.float32)
    q = dh // 4
    sgn = np.concatenate([-np.ones(q), np.ones(q), -np.ones(q), np.ones(q)]).astype(np.float32)
    return cos, (sin * sgn[None, :]).astype(np.float32)


def _band_masks(j):
    kp = np.arange(128)[:, None]
    qf = np.arange(128)[None, :]
    m = np.zeros((128, 8, 2, 128), np.float32)
    for n in range(8):
        gn = 8 * j + n
        if gn - 1 >= 0:
            m[:, n, 0, :] = (qf <= kp)
        if gn + 1 <= 31:
            m[:, n, 1, :] = (kp <= qf)
    return m.astype(NPBF)


def kernel(**inp):
    inp = {k: np.ascontiguousarray(np.asarray(v)) for k, v in inp.items()}
    if "nc" not in _NC:
        _NC["nc"] = build_fused()
    nc = _NC["nc"]
    cos128, sin128 = _rope_tables(128)
    cos64, sin64 = _rope_tables(64)
    wm_all = np.concatenate([inp["w_mod"][0], inp["w_mod"][1]], axis=1)
    bm_all = np.concatenate([inp["b_mod"][0], inp["b_mod"][1]], axis=0)
    shared = dict(
        norm_mix=inp["norm_mix"], norm_ffn=inp["norm_ffn"], w_in=inp["w_in"],
        g128=np.ascontiguousarray(np.stack([inp["qn_a"], inp["kn_a"], inp["qn_c"], inp["kn_c"]], axis=1)),
        g64=np.ascontiguousarray(np.stack([inp["qn_b"], inp["kn_b"]], axis=1)),
        ident=_IDENT_B, identf=_IDENT_F,
        w_br_a=inp["w_br_a"], w_br_b=inp["w_br_b"], w_br_c=inp["w_br_c"], w_out=inp["w_out"], w_router=inp["w_router"],
        lamv=np.ascontiguousarray(np.stack([inp["lam_q1"], inp["lam_k1"], inp["lam_q2"], inp["lam_k2"]], axis=1)),
        subln=inp["subln_b"], sink=inp["sink_c"])
    maps = []
    for i in range(NCORE):
        s, q = i // 4, i % 4
        cst = np.stack([inp["c"][s], inp["c_ctx"]], axis=0)
        cT = np.ascontiguousarray(cst.T.reshape(16, 128, 2).transpose(1, 0, 2))
        sl = slice(q * TL, (q + 1) * TL)
        widx = np.zeros((128, 10), np.uint32)
        for blk in range(10):
            gb = min(max(8 * q - 1 + blk, 0), 31)
            widx[:, blk] = gb * 128 + np.arange(128)
        arow = np.array([[qq * NEXP + 4 * q + el] for el in range(NEL) for qq in range(4)], np.uint32)
        m = dict(shared)
        m.update(
            xin=np.ascontiguousarray(np.concatenate([inp["x"][s, sl], inp["ctx"][s, q * TC:(q + 1) * TC]], 0)),
            cT=cT, wm=np.ascontiguousarray(wm_all[:, q * MW:(q + 1) * MW]), bm=np.ascontiguousarray(bm_all[q * MW:(q + 1) * MW]),
            cos128=cos128[sl], sin128=sin128[sl], cos64=cos64[sl], sin64=sin64[sl],
            bandm=_band_masks(q), widx=widx, arow=arow,
            wg=np.ascontiguousarray(inp["w_gate"][:, 4 * q:4 * q + 4]), wu=np.ascontiguousarray(inp["w_up"][:, 4 * q:4 * q + 4]),
            wd=np.ascontiguousarray(inp["w_down"][:, 4 * q:4 * q + 4]))
        maps.append(m)
    res = run_bass_kernel_spmd(nc, maps, core_ids=list(range(NCORE)))
    outs = [np.asarray(r["out"]) for r in res.results]
    out = np.stack([np.concatenate(outs[4 * s:4 * s + 4], 0) for s in range(2)])
    return out.astype(np.float32)
```

```python
import contextlib
import math
import numpy as np
import ml_dtypes
import concourse.bass as bass
import concourse.mybir as mybir
from concourse.bass_utils import run_bass_kernel_spmd

F32 = mybir.dt.float32
BF16 = mybir.dt.bfloat16
I32 = mybir.dt.int32
U32 = mybir.dt.uint32
ALU = mybir.AluOpType
AF = mybir.ActivationFunctionType
AX = mybir.AxisListType
NPBF = ml_dtypes.bfloat16

D = 2048
SEQ = 4096
CTX = 256
NCORE = 8
TL = 1024
TC = 64
TT = TL + TC
KEYS = SEQ + CTX
NKB = KEYS // 128
IN_W = 10240
EPS = 1e-6
NEXP = 16
FF = 1024
CAP_L = 512
CAP_C = 32

ENGS = ("sync", "scalar", "vector", "gpsimd", "tensor")
NDMA_SEM = 12
CC_OVERLAP = False
H2_SPREAD = True
KV_SPREAD = True


class Prog:
    def __init__(self, nc):
        self.nc = nc
        self.q = {e: [] for e in ENGS}
        self.cnt = {e: 0 for e in ENGS}
        self.waited = {e: {} for e in ENGS}
        self.res = {}
        self.dma_i = {e: 0 for e in ENGS}
        self.dma_cnt = {}
        self.final_tokens = []

    def barrier(self):
        bar = [(("eng", e), c) for e, c in self.cnt.items() if c > 0]
        for key, c in self.dma_cnt.items():
            bar.append((key, c if key[0] == "cc" else 16 * c))
        self.bar = bar

    def op(self, eng, fn, reads=(), writes=(), dma=False, final=False, cc=False):
        deps = list(getattr(self, "bar", ()))
        for r in reads:
            st = self.res.get(r)
            if st and st["w"]:
                deps.append(st["w"])
        for r in writes:
            st = self.res.get(r)
            if st:
                if st["w"]:
                    deps.append(st["w"])
                deps.extend(st["r"].items())
        if cc:
            key = ("cc", eng, 0)
            c = self.dma_cnt.get(key, 0)
            if c > 0 and not CC_OVERLAP:
                deps.append((key, c))
            self.dma_cnt[key] = c + 1
            token = (key, c + 1)
            inc = 1
        elif dma:
            j = self.dma_i[eng] % NDMA_SEM
            self.dma_i[eng] += 1
            key = ("dma", eng, j)
            c = self.dma_cnt.get(key, 0)
            if c > 0:
                deps.append((key, 16 * c))
            self.dma_cnt[key] = c + 1
            token = (key, 16 * (c + 1))
            inc = 16
        else:
            self.cnt[eng] += 1
            key = ("eng", eng)
            token = (key, self.cnt[eng])
            inc = 1
        waits = {}
        for (k, v) in deps:
            if k == ("eng", eng) and eng == "tensor":
                continue
            if self.waited[eng].get(k, 0) >= v:
                continue
            if waits.get(k, 0) < v:
                waits[k] = v
        for k, v in waits.items():
            self.waited[eng][k] = v
        self.q[eng].append((list(waits.items()), fn, key, inc))
        for r in reads:
            st = self.res.setdefault(r, {"w": None, "r": {}})
            if st["r"].get(token[0], 0) < token[1]:
                st["r"][token[0]] = token[1]
        for r in writes:
            self.res[r] = {"w": token, "r": {}}
        if final:
            self.final_tokens.append(token)
        return token

    def emit(self):
        nc = self.nc
        keys = set()
        for e in ENGS:
            for (w, fn, key, inc) in self.q[e]:
                keys.add(key)
        with contextlib.ExitStack() as es:
            sems = {}
            for k in sorted(keys):
                sems[k] = es.enter_context(nc.semaphore("s_" + "_".join(str(x) for x in k)))
            block = es.enter_context(nc.Block())
            finals = self.final_tokens

            def make(ename):
                def body(engine):
                    for (w, fn, key, inc) in self.q[ename]:
                        for (k, v) in w:
                            engine.wait_ge(sems[k], v)
                        ins = fn(engine)
                        ins.then_inc(sems[key], inc)
                    if ename == "sync":
                        for (k, v) in finals:
                            engine.wait_ge(sems[k], v)
                return body

            for e in ENGS:
                if self.q[e] or e == "sync":
                    getattr(block, e)(make(e))


ARENA_B = 204800
_ISZ = {F32: 4, U32: 4, I32: 4, BF16: 2}


class State:
    def __init__(self, nc, es):
        self.nc = nc
        self.P = Prog(nc)
        self.arena = es.enter_context(nc.sbuf_tensor("arena", [128, ARENA_B // 2], BF16))
        self.psum = es.enter_context(nc.psum_tensor("psum", [128, 4096], F32))
        self.v = {BF16: self.arena[:], F32: self.arena[:].bitcast(F32), U32: self.arena[:].bitcast(U32)}
        self.pv = {F32: self.psum[:], BF16: self.psum[:].bitcast(BF16)}
        self.off = 0
        self.poff = 0
        self.nd = 0

    def phase(self):
        self.off = 0
        self.poff = 0
        self.P.barrier()

    @staticmethod
    def _shape_view(view, shape):
        if len(shape) == 2:
            return view
        names = ["a%d" % i for i in range(len(shape) - 1)]
        kw = {n: s for n, s in zip(names, shape[1:])}
        return view.rearrange("p (%s) -> p %s" % (" ".join(names), " ".join(names)), **kw)

    def sb(self, shape, dt, name=None):
        isz = _ISZ[dt]
        n = int(np.prod(shape[1:]))
        nb = (n * isz + 31) // 32 * 32
        assert self.off + nb <= ARENA_B, ("SBUF arena overflow", self.off, nb)
        o = self.off // isz
        self.off += nb
        return self._shape_view(self.v[dt][:shape[0], o:o + n], list(shape))

    def ps(self, shape, dt, name=None):
        isz = _ISZ[dt]
        n = int(np.prod(shape[1:]))
        nb = (n * isz + 2047) // 2048 * 2048
        assert self.poff + nb <= 16384, "PSUM overflow"
        o = self.poff // isz
        self.poff += nb
        return self._shape_view(self.pv[dt][:shape[0], o:o + n], list(shape))

    def dram(self, shape, dt, name=None):
        self.nd += 1
        return self.nc.dram_tensor(name or ("scr%d" % self.nd), list(shape), dt).ap()


def _din(nc, name, shape, dt):
    return nc.dram_tensor(name, list(shape), dt, kind="ExternalInput").ap()


def _dout(nc, name, shape, dt):
    return nc.dram_tensor(name, list(shape), dt, kind="ExternalOutput").ap()


RG = [[0, 1, 2, 3], [4, 5, 6, 7]]


def _cc(P, kind, op, src, dst, reads, writes):
    P.op("gpsimd", lambda e: e.collective_compute(kind, op, replica_groups=RG, ins=[src.opt()], outs=[dst.opt()]),
         reads=reads, writes=writes, cc=True)


MW = 6144


def phase_M(S, T):
    P = S.P
    S.phase()
    ct = S.sb([128, 16, 2], F32)
    st = S.sb([128, 16, 2], F32)
    bt = S.sb([2, MW], F32)
    ot = S.sb([2, MW], F32)
    ws = [S.sb([128, 16, 512], F32) for _ in range(2)]
    pm = [S.ps([128, 512], F32) for _ in range(2)]
    P.op("sync", lambda e: e.dma_start(out=ct[:], in_=T["cT"]), writes=["ct"], dma=True)
    P.op("sync", lambda e: e.dma_start(out=bt[:], in_=T["bm"].partition_broadcast(2)), writes=["bt"], dma=True)
    P.op("scalar", lambda e: e.activation(out=st[:], in_=ct[:], func=AF.Silu), reads=["ct"], writes=["st"])
    for g in range(MW // 512):
        s = g % 2
        P.op("sync" if g % 2 == 0 else "gpsimd",
             lambda e, g=g, s=s: e.dma_start(out=ws[s][:], in_=T["wm"][:, g * 512:(g + 1) * 512].rearrange("(k p) c -> p k c", p=128)),
             writes=["ws%d" % s], dma=True)
        for k in range(16):
            P.op("tensor", lambda e, k=k, s=s: e.matmul(pm[s][0:2, :], lhsT=st[:, k, :], rhs=ws[s][:, k, :], start=(k == 0), stop=(k == 15)),
                 reads=["st", "ws%d" % s], writes=["pm%d" % s])
        P.op("vector", lambda e, g=g, s=s: e.tensor_tensor(out=ot[:, g * 512:(g + 1) * 512], in0=pm[s][0:2, :], in1=bt[:, g * 512:(g + 1) * 512], op=ALU.add),
             reads=["pm%d" % s, "bt"], writes=["ot"])
    P.op("sync", lambda e: e.dma_start(out=T["modloc"], in_=ot[:]), reads=["ot"], writes=["modloc"], dma=True)
    _cc(P, "AllGather", ALU.bypass, T["modloc"], T["modG"], ["modloc"], ["modG"])


def modv(T, l, r, ch):
    row = (2 * l + ch // 3) * 2 + r
    c0 = (ch % 3) * D
    return T["modG"][row, c0:c0 + D]


SEGS = {
    0: [(0, 256, "n128", "KTA", 0, 1), (256, 256, "v", 0)],
    1: [(0, 512, "n64", "KTB", 0, 1)],
    2: [(0, 512, "v", 256)],
    3: [(0, 256, "n128", "KTC", 0, 3), (256, 256, "v", 768)],
    4: [(0, 512, "n128", "QTA", 0, 0)],
    5: [(0, 256, "n128", "QTA", 4, 0), (256, 256, "n64", "QTB", 0, 0)],
    6: [(0, 256, "n64", "QTB", 4, 0), (256, 256, "n128", "QTC", 0, 2)],
    7: [(0, 512, "n128", "QTC", 2, 2)],
}
DEST = {"KTA": ("K0", 0), "KTC": ("K0", 256), "KTB": ("K1", 0), "QTA": ("Q", 0), "QTB": ("Q", 768), "QTC": ("Q", 1280)}


def phase_A(S, T, l, combine, proj, nt, xsrc, xdst, final_out=False):
    P = S.P
    S.phase()
    L = T["L"][l] if proj else None
    if combine:
        Lp = T["L"][l - 1]

    def rows(t):
        return 128 if t < 8 else TC

    xt = [S.sb([128, D], F32) for _ in range(2)]
    if combine:
        acc = S.sb([128, D], F32)
        g2t = S.sb([128, D], F32)
    if proj:
        amul = S.sb([128, D], F32)
        bsh = S.sb([128, D], F32)
        sqs = S.sb([128, D], F32)
        ss = S.sb([128, 2], F32)
        epst = S.sb([128, 1], F32)
        hb = [S.sb([128, D], BF16) for _ in range(2)]
        hT = S.sb([128, 16, TT], BF16)
        idt = S.sb([128, 128], BF16)
        gt128 = S.sb([128, 4, 128], F32)
        gt64 = S.sb([128, 2, 64], F32)
        c128 = S.sb([128, 8, 128], F32)
        s128 = S.sb([128, 8, 128], F32)
        c64 = S.sb([128, 8, 64], F32)
        s64 = S.sb([128, 8, 64], F32)
        wsl = [S.sb([128, 16, 512], BF16) for _ in range(2)]
        NS = 4
        sq2 = [S.sb([128, 512], F32) for _ in range(NS)]
        ssh = [S.sb([128, 8], F32) for _ in range(NS)]
        yf = [S.sb([128, 512], F32) for _ in range(NS)]
        t1 = [S.sb([128, 512], F32) for _ in range(NS)]
        t2 = [S.sb([128, 512], F32) for _ in range(NS)]
        yb = [S.sb([128, 512], BF16) for _ in range(NS)]
        qk = [S.sb([128, 8, 128], BF16) for _ in range(4)]
        ob = [S.sb([128, 512], BF16) for _ in range(6)]
        NPA = 5
        pacc = [S.ps([128, 512], F32) for _ in range(NPA)]
        pT = S.pv[BF16][:, 0:2048].rearrange("p (k t) -> p k t", k=16)
        pTh = [S.ps([128, 8, 128], BF16) for _ in range(2)]
        P.op("sync", lambda e: e.dma_start(out=idt[:], in_=T["ident"]), writes=["idt"], dma=True)
        P.op("sync", lambda e: e.dma_start(out=gt128[:], in_=T["g128"][l].partition_broadcast(128)), writes=["gt128"], dma=True)
        P.op("sync", lambda e: e.dma_start(out=gt64[:], in_=T["g64"][l].partition_broadcast(128)), writes=["gt64"], dma=True)
        for (dst, srcn, nm) in ((c128, "cos128", "c128"), (s128, "sin128", "s128"), (c64, "cos64", "c64"), (s64, "sin64", "s64")):
            P.op("sync", lambda e, dst=dst, srcn=srcn: e.dma_start(out=dst[:], in_=T[srcn].rearrange("(t p) d -> p t d", p=128)), writes=[nm], dma=True)
        P.op("vector", lambda e: e.memset(epst[:], EPS), writes=["epst"])

    def load_mod(which):
        P.op("sync", lambda e: e.dma_start(out=amul[:], in_=modv(T, l, which, 1).partition_broadcast(128)), writes=["amul"], dma=True)
        P.op("sync", lambda e: e.dma_start(out=bsh[:], in_=T["norm_mix"][l].partition_broadcast(128)), writes=["bsh"], dma=True)
        P.op("vector", lambda e: e.scalar_tensor_tensor(out=amul[:], in0=amul[:], scalar=1.0, in1=bsh[:], op0=ALU.add, op1=ALU.mult),
             reads=["amul", "bsh"], writes=["amul"])
        P.op("sync", lambda e: e.dma_start(out=bsh[:], in_=modv(T, l, which, 0).partition_broadcast(128)), reads=["bsh"], writes=["bsh"], dma=True)

    def load_g2(which):
        P.op("sync", lambda e: e.dma_start(out=g2t[:], in_=modv(T, l - 1, which, 5).partition_broadcast(128)), writes=["g2t"], dma=True)

    if proj:
        load_mod(0)
    if combine:
        load_g2(0)
    for t in range(nt):
        r = rows(t)
        r0 = t * 128
        xs = t % 2
        xk = "xt%d" % xs
        if t == 8:
            if proj:
                load_mod(1)
            if combine:
                load_g2(1)
        P.op("sync", lambda e, xs=xs, r=r, r0=r0: e.dma_start(out=xt[xs][:r, :], in_=xsrc[r0:r0 + r, :]), writes=[xk], dma=True)
        if combine:
            csrc = Lp["comb%d" % (t // 2)][(t % 2) * 128:(t % 2) * 128 + r, :] if t < 8 else Lp["comb_ctx"][0:r, :]
            P.op("gpsimd", lambda e, r=r, csrc=csrc: e.dma_start(out=acc[:r, :], in_=csrc), writes=["acc"], dma=True)
            P.op("gpsimd", lambda e, r=r: e.tensor_tensor(out=acc[:r, :], in0=acc[:r, :], in1=g2t[:r, :], op=ALU.mult), reads=["acc", "g2t"], writes=["acc"])
            P.op("vector", lambda e, xs=xs, r=r: e.tensor_tensor(out=xt[xs][:r, :], in0=xt[xs][:r, :], in1=acc[:r, :], op=ALU.add), reads=["acc", xk], writes=[xk])
            P.op("sync", lambda e, xs=xs, r=r, r0=r0: e.dma_start(out=xdst[r0:r0 + r, :], in_=xt[xs][:r, :]), reads=[xk], dma=True, final=final_out)
        if not proj:
            continue
        hs = t % 2
        hk = "hb%d" % hs
        P.op("scalar", lambda e, xs=xs, hs=hs, r=r: e.activation(out=hb[hs][:r, :], in_=xt[xs][:r, :], func=AF.Square, accum_out=ss[:r, 0:1]),
             reads=[xk], writes=[hk, "ss"])
        P.op("scalar", lambda e, r=r: e.activation(out=ss[:r, 1:2], in_=ss[:r, 0:1], func=AF.Sqrt, bias=epst[:r, :], scale=1.0 / D),
             reads=["ss", "epst"], writes=["ss1"])
        P.op("vector", lambda e, r=r: e.reciprocal(out=ss[:r, 1:2], in_=ss[:r, 1:2]), reads=["ss1"], writes=["ss1"])
        P.op("vector", lambda e, xs=xs, r=r: e.scalar_tensor_tensor(out=sqs[:r, :], in0=xt[xs][:r, :], scalar=ss[:r, 1:2], in1=amul[:r, :], op0=ALU.mult, op1=ALU.mult),
             reads=[xk, "ss1", "amul", "sqs"], writes=["sqs"])
        P.op("gpsimd", lambda e, hs=hs, r=r: e.tensor_tensor(out=hb[hs][:r, :], in0=sqs[:r, :], in1=bsh[:r, :], op=ALU.add),
             reads=["sqs", "bsh"], writes=[hk])
        for k in range(16):
            P.op("tensor", lambda e, k=k, hs=hs, r=r: e.transpose(out=pT[:, k, :r], in_=hb[hs][:r, k * 128:(k + 1) * 128], identity=idt[:r, :r]),
                 reads=[hk, "idt"], writes=["pacc%d" % (k // 8)])
        P.op("scalar", lambda e, r=r, r0=r0: e.copy(out=hT[:, :, r0:r0 + r], in_=pT[:, :, :r]), reads=["pacc0", "pacc1"], writes=["hT%d" % t])

    if not proj:
        return

    w_in = T["w_in"][l]
    u = 0
    nseg = 0
    pending = []
    KVK = ["ccKV%d" % j_ for j_ in range(7)]
    TAILC = [0]
    ZC = [0]
    def load_w(g):
        s_ = g % 2
        P.op("gpsimd", lambda e, g=g, s_=s_: e.dma_start(out=wsl[s_][:], in_=w_in[:, g * 512:(g + 1) * 512].rearrange("(k p) c -> p k c", p=128)),
             writes=["wsl%d" % s_], dma=True)

    KVPAIRS = (("KTloc0", "KTG0"), ("Vloc0", "VG0"), ("KTloc1", "KTG1"), ("Vloc1", "VG1"), ("WKVloc", "WG"), ("KTlocC", "KTGc"), ("VlocC", "VGc"))

    def share_kv():
        for j_, (a, b) in enumerate(KVPAIRS):
            _cc(P, "AllGather", ALU.bypass, L[a], L[b], [], [b, KVK[j_]])

    load_w(0)
    for g in range(20):
        s = g % 2
        wk = "wsl%d" % s
        if g + 1 < 20:
            load_w(g + 1)
        if KV_SPREAD and 4 <= g < 4 + len(KVPAIRS):
            if g == 4:
                while pending:
                    for f_ in pending.pop(0):
                        f_()
            a_, b_ = KVPAIRS[g - 4]
            _cc(P, "AllGather", ALU.bypass, L[a_], L[b_], [], [b_, KVK[g - 4]])
        for t in range(nt):
            r = rows(t)
            r0 = t * 128
            lat = t < 8
            n_this = sum(1 for sg in SEGS.get(g, []) if sg[2] != "v")
            while pending and sum(len(x) for x in pending) + n_this > NS:
                for f_ in pending.pop(0):
                    f_()
            pb = u % NPA
            u += 1
            pk = "pacc%d" % pb
            for k in range(16):
                P.op("tensor", lambda e, k=k, s=s, pb=pb, r=r, r0=r0: e.matmul(pacc[pb][:r, :], lhsT=hT[:, k, r0:r0 + r], rhs=wsl[s][:, k, :], start=(k == 0), stop=(k == 15)),
                     reads=["hT%d" % t, wk], writes=[pk])
            unit_tails = []
            if g >= 8:
                o = nseg % 6
                nseg += 1
                ok = "ob%d" % o
                P.op("scalar", lambda e, pb=pb, o=o, r=r: e.activation(out=ob[o][:r, :], in_=pacc[pb][:r, :], func=AF.Sigmoid), reads=[pk], writes=[ok])
                P.op("sync", lambda e, o=o, r=r, r0=r0, g=g: e.dma_start(out=L["G"][r0:r0 + r, (g - 8) * 512:(g - 7) * 512], in_=ob[o][:r, :]), reads=[ok], dma=True)
                if pending:
                    for f_ in pending.pop(0):
                        f_()
                continue
            for seg in SEGS[g]:
                c0, w, kind = seg[0], seg[1], seg[2]
                if kind == "v":
                    o = nseg % 6
                    nseg += 1
                    ok = "ob%d" % o
                    vc = seg[3]
                    P.op("scalar", lambda e, pb=pb, o=o, r=r, c0=c0, w=w: e.copy(out=ob[o][:r, :w], in_=pacc[pb][:r, c0:c0 + w]), reads=[pk], writes=[ok])
                    vdst = L["Vloc%d" % (t // 4)][(t % 4) * 128:(t % 4) * 128 + r, vc:vc + w] if lat else L["VlocC"][0:r, vc:vc + w]
                    P.op("sync", lambda e, o=o, r=r, w=w, vdst=vdst: e.dma_start(out=vdst, in_=ob[o][:r, :w]), reads=[ok] + KVK, dma=True)
                    if vc == 768 and lat:
                        P.op("sync", lambda e, o=o, r=r, r0=r0, w=w: e.dma_start(out=L["WKVloc"][r0:r0 + r, 0:256], in_=ob[o][:r, :w]), reads=[ok] + KVK, dma=True)
                    continue
                dest, h0, gi = seg[3], seg[4], seg[5]
                d = 128 if kind == "n128" else 64
                H = w // d
                z = ZC[0] % NS
                ZC[0] += 1
                nseg += 1
                zk = "z%d" % z
                pv = pacc[pb][:r, c0:c0 + w].rearrange("p (h d) -> p h d", d=d)
                P.op("scalar", lambda e, pb=pb, z=z, r=r, c0=c0, w=w: e.activation(out=sq2[z][:r, :w], in_=pacc[pb][:r, c0:c0 + w], func=AF.Square),
                     reads=[pk], writes=[zk + "sq"])
                P.op("vector", lambda e, z=z, r=r, w=w, H=H, d=d: e.tensor_reduce(out=ssh[z][:r, :H], in_=sq2[z][:r, :w].rearrange("p (h d) -> p h d", d=d), axis=AX.X, op=ALU.add),
                     reads=[zk + "sq"], writes=[zk + "ss"])
                P.op("scalar", lambda e, z=z, r=r, H=H, d=d: e.activation(out=ssh[z][:r, :H], in_=ssh[z][:r, :H], func=AF.Sqrt, bias=epst[:r, :], scale=1.0 / d),
                     reads=[zk + "ss", "epst"], writes=[zk + "ss"])
                P.op("vector", lambda e, z=z, r=r, H=H: e.reciprocal(out=ssh[z][:r, :H], in_=ssh[z][:r, :H]), reads=[zk + "ss"], writes=[zk + "ss"])
                y3 = yf[z][:r, :w].rearrange("p (h d) -> p h d", d=d)
                P.op("vector", lambda e, pv=pv, y3=y3, z=z, r=r, H=H, d=d: e.tensor_tensor(out=y3, in0=pv, in1=ssh[z][:r, :H].unsqueeze(2).to_broadcast([r, H, d]), op=ALU.mult),
                     reads=[pk, zk + "ss"], writes=[zk + "y"])
                gtile = (gt128[:r, gi, :] if d == 128 else gt64[:r, gi, :])
                if lat:
                    P.op("vector", lambda e, y3=y3, gtile=gtile, r=r, H=H, d=d: e.tensor_tensor(out=y3, in0=y3, in1=gtile.unsqueeze(1).to_broadcast([r, H, d]), op=ALU.mult),
                         reads=[zk + "y", "gt128", "gt64"], writes=[zk + "y"])
                    ct_, st_ = (c128, s128) if d == 128 else (c64, s64)
                    e4 = d // 4
                    t13 = t1[z][:r, :w].rearrange("p (h d) -> p h d", d=d)
                    P.op("vector", lambda e, y3=y3, t13=t13, ct_=ct_, t=t, r=r, H=H, d=d: e.tensor_tensor(out=t13, in0=y3, in1=ct_[:r, t, :].unsqueeze(1).to_broadcast([r, H, d]), op=ALU.mult),
                         reads=[zk + "y", "c128", "c64"], writes=[zk + "t1"])
                    y5 = yf[z][:r, :w].rearrange("p (h a b e) -> p h a b e", h=H, a=2, b=2, e=e4)
                    t25 = t2[z][:r, :w].rearrange("p (h a b e) -> p h a b e", h=H, a=2, b=2, e=e4)
                    s4 = st_[:r, t, :].rearrange("p (a b e) -> p a b e", a=2, b=2, e=e4)
                    for bsel in (0, 1):
                        P.op("vector",
                             lambda e, y5=y5, t25=t25, s4=s4, bsel=bsel, r=r, H=H, e4=e4: e.tensor_tensor(out=t25[:, :, :, bsel, :], in0=y5[:, :, :, 1 - bsel, :], in1=s4[:, :, bsel, :].unsqueeze(1).to_broadcast([r, H, 2, e4]), op=ALU.mult),
                             reads=[zk + "y", "s128", "s64"], writes=[zk + "t2%d" % bsel])
                    P.op("vector", lambda e, z=z, r=r, w=w: e.tensor_tensor(out=yb[z][:r, :w], in0=t1[z][:r, :w], in1=t2[z][:r, :w], op=ALU.add),
                         reads=[zk + "t1", zk + "t20", zk + "t21"], writes=[zk + "yb"])
                    if dest == "KTC":
                        P.op("sync", lambda e, z=z, r=r, r0=r0, w=w: e.dma_start(out=L["WKVloc"][r0:r0 + r, 256:512], in_=yb[z][:r, :w]), reads=[zk + "yb"] + KVK, dma=True)
                else:
                    yb3 = yb[z][:r, :w].rearrange("p (h d) -> p h d", d=d)
                    P.op("vector", lambda e, y3=y3, yb3=yb3, gtile=gtile, r=r, H=H, d=d: e.tensor_tensor(out=yb3, in0=y3, in1=gtile.unsqueeze(1).to_broadcast([r, H, d]), op=ALU.mult),
                         reads=[zk + "y", "gt128", "gt64"], writes=[zk + "yb"])
                kind_, base = DEST[dest]
                f0 = base + h0 * d
                if kind_ == "Q":
                    dd = L["QT"][f0:f0 + H * d, r0:r0 + r]
                elif lat:
                    dd = L["KTloc0" if kind_ == "K0" else "KTloc1"][f0:f0 + H * d, r0:r0 + r]
                else:
                    fc = f0 + (512 if kind_ == "K1" else 0)
                    dd = L["KTlocC"][fc:fc + H * d, 0:r]

                def tail(z=z, zk=zk, r=r, H=H, d=d, dd=dd, isk=(kind_ != "Q")):
                    tc_ = TAILC[0]
                    TAILC[0] += 1
                    ph = tc_ % 2
                    phk = "pTh%d" % ph
                    for j in range(H):
                        P.op("tensor", lambda e, j=j: e.transpose(out=pTh[ph][:d, j, :r], in_=yb[z][:r, j * d:(j + 1) * d], identity=idt[:r, :r]),
                             reads=[zk + "yb", "idt"], writes=[phk])
                    qs = tc_ % 4
                    qkk = "qk%d" % qs
                    P.op("scalar", lambda e: e.copy(out=qk[qs][:d, :H, :r], in_=pTh[ph][:d, :H, :r]), reads=[phk], writes=[qkk])
                    P.op("sync", lambda e: e.dma_start(out=dd.rearrange("(h d) t -> d h t", d=d), in_=qk[qs][:d, :H, :r]), reads=[qkk] + (KVK if isk else []), dma=True)

                unit_tails.append(tail)
            pending.append(unit_tails)
    for ut in pending:
        for f_ in ut:
            f_()
    if not KV_SPREAD:
        S.P.barrier()
        share_kv()
ARENA = 36864
ACC_DEN = False


def phase_B(S, T, l, ntiles, xres):
    layer = l
    lam_init = 0.8 - 0.6 * math.exp(-0.3 * layer)
    has_ctx = ntiles == 9
    ntq = TL + (TC if has_ctx else 0)
    P = S.P
    S.phase()
    L = T["L"][l]
    Gin = L["G"]
    xin = xres
    w_br_a, w_br_b, w_br_c, w_out, w_router = T["w_br_a"][l], T["w_br_b"][l], T["w_br_c"][l], T["w_out"][l], T["w_router"][l]
    normf = T["norm_ffn"][l]
    lamv, subln, sink = T["lamv"][l], T["subln"][l], T["sink"][l]
    ident, identf, bandm = T["ident"], T["identf"], T["bandm"]
    x1o = L["x1"]
    KTG0, KTG1, KTGc, VG0, VG1, VGc, WG, QT = L["KTG0"], L["KTG1"], L["KTGc"], L["VG0"], L["VG1"], L["VGc"], L["WG"], L["QT"]
    VGs = [VG0, VG1]

    def rows(t):
        return 128 if t < 8 else TC

    if True:
        C = S
        abase = S.off
        arena = S.sb([128, ARENA], BF16)
        qar = C.sb([128, 6 * ntq], BF16)
        OT = C.sb([128, 16, ntq], BF16)
        ptl = [C.sb([128, 512], BF16) for _ in range(6)]
        rec = C.sb([128, 512], F32)
        oa = C.sb([128, 512], F32)
        obb = C.sb([128, 512], F32)
        od = C.sb([128, 512], F32)
        osq = C.sb([128, 512], BF16)
        ones = C.sb([128, 128], BF16)
        onesf = C.sb([128, 128], F32)
        accD = [[C.sb([128, 512], F32) for _ in range(2)] for _ in range(2)]
        idt = C.sb([128, 128], BF16)
        idf = C.sb([128, 128], F32)
        bm = C.sb([128, 8, 2, 128], BF16)
        widx = C.sb([128, 10], U32)
        tl = C.sb([128, 4, 64], F32)
        tl2 = C.sb([128, 2, 64], F32)
        lsc = C.sb([128, 4], F32)
        subs = C.sb([128, 1], F32)
        esink = C.sb([128, 6], F32)
        epst = C.sb([128, 1], F32)
        fsc = C.sb([128, 1], F32)
        wr = C.sb([128, 16, NEXP], F32)
        g1t = [C.sb([128, D], F32) for _ in range(2)]
        xsl = [C.sb([128, 512], F32) for _ in range(2)]
        gsl = [C.sb([128, 3, 512], BF16) for _ in range(2)]
        mtmp = [C.sb([128, 512], F32) for _ in range(2)]
        mb = [C.sb([128, 512], BF16) for _ in range(2)]
        sm = C.sb([128, 4], F32)
        afft = [C.sb([128, NEXP], F32) for _ in range(2)]
        affTs = [C.sb([16, 128], F32) for _ in range(2)]
        bank = [C.ps([128, 512], F32) for i in range(8)]
        pTb = S.pv[BF16][:, 7 * 1024:7 * 1024 + 512].rearrange("p (c t) -> p c t", c=4)

        P.op("sync", lambda e: e.dma_start(out=idt[:], in_=ident), writes=["idt"], dma=True)
        P.op("sync", lambda e: e.dma_start(out=idf[:], in_=identf), writes=["idf"], dma=True)
        P.op("sync", lambda e: e.dma_start(out=bm[:], in_=bandm), writes=["bm"], dma=True)
        P.op("sync", lambda e: e.dma_start(out=widx[:], in_=T["widx"]), writes=["widx"], dma=True)
        P.op("sync", lambda e: e.dma_start(out=tl[:], in_=lamv.partition_broadcast(128)), writes=["tl"], dma=True)
        P.op("sync", lambda e: e.dma_start(out=subs[:], in_=subln.rearrange("(p o) -> p o", o=1)), writes=["subs"], dma=True)
        P.op("sync", lambda e: e.dma_start(out=esink[:], in_=sink.partition_broadcast(128)), writes=["esink"], dma=True)
        P.op("sync", lambda e: e.dma_start(out=wr[:], in_=w_router.rearrange("(k p) n -> p k n", p=128)), writes=["wr"], dma=True)
        for w_ in (0, 1):
            P.op("sync", lambda e, w_=w_: e.dma_start(out=g1t[w_][:], in_=modv(T, l, w_, 2).partition_broadcast(128)), writes=["g1t%d" % w_], dma=True)
        P.op("vector", lambda e: e.memset(ones[:], 1.0), writes=["ones"])
        P.op("vector", lambda e: e.memset(onesf[:], 1.0), writes=["onesf"])
        P.op("vector", lambda e: e.memset(epst[:], EPS), writes=["epst"])
        P.op("vector", lambda e: e.memset(fsc[:], 0.0), writes=["fsc"])
        tl4 = tl[:].rearrange("p (a b) d -> p a b d", b=2)
        P.op("vector", lambda e: e.tensor_tensor(out=tl2[:], in0=tl4[:, :, 0, :], in1=tl4[:, :, 1, :], op=ALU.mult), reads=["tl"], writes=["tl2"])
        P.op("vector", lambda e: e.tensor_reduce(out=lsc[:, 0:2], in_=tl2[:], axis=AX.X, op=ALU.add), reads=["tl2"], writes=["lsc"])
        P.op("scalar", lambda e: e.activation(out=lsc[:, 0:2], in_=lsc[:, 0:2], func=AF.Exp), reads=["lsc"], writes=["lsc"])
        P.op("vector", lambda e: e.tensor_tensor(out=lsc[:, 2:3], in0=lsc[:, 1:2], in1=lsc[:, 0:1], op=ALU.subtract), reads=["lsc"], writes=["lsc2"])
        P.op("vector", lambda e: e.tensor_scalar(out=lsc[:, 3:4], in0=lsc[:, 2:3], scalar1=-lam_init, scalar2=None, op0=ALU.add), reads=["lsc2"], writes=["neglam"])
        P.op("vector", lambda e: e.tensor_scalar(out=subs[:], in0=subs[:], scalar1=1.0 - lam_init, scalar2=None, op0=ALU.mult), reads=["subs"], writes=["subs"])
        P.op("scalar", lambda e: e.activation(out=esink[:], in_=esink[:], func=AF.Exp), reads=["esink"], writes=["esink"])

        arena_keys = []

        def fence(new_keys):
            P.op("gpsimd", lambda e: e.memset(fsc[:], 0.0), writes=["fsc"] + arena_keys + new_keys)
            arena_keys[:] = list(new_keys)

        state = {"psi": 0, "pti": 0, "ui": 0}

        def run_units(units):
            steps = []
            for u in units:
                u["uid"] = state["ui"]
                state["ui"] += 1
                nk = len(u["kblocks"])
                u["accm"] = ACC_DEN and nk >= 8
                u["nacc"] = 0
                for i, kb in enumerate(u["kblocks"]):
                    steps.append((u, kb, i == 0, i == nk - 1))
            LA = 3
            slots = {}
            for i in range(len(steps) + LA):
                if i < len(steps):
                    u, (kT, v, mask), first, last = steps[i]
                    nq = u["nq"]
                    b = state["psi"] % 4
                    state["psi"] += 1
                    p = state["pti"] % 6
                    state["pti"] += 1
                    slots[i] = p
                    bk = "bank%d" % b
                    P.op("tensor", lambda e, kT=kT, q=u["q"], b=b, nq=nq: e.matmul(bank[b][:, :nq], lhsT=kT, rhs=q, start=True, stop=True),
                         reads=u["rk"], writes=[bk])
                    P.op("scalar", lambda e, b=b, p=p, nq=nq, sc=u["scale"]: e.activation(out=ptl[p][:, :nq], in_=bank[b][:, :nq], func=AF.Exp, scale=sc),
                         reads=[bk], writes=["ptl%d" % p])
                    if mask is not None:
                        P.op("vector", lambda e, p=p, nq=nq, mask=mask: e.tensor_tensor(out=ptl[p][:, :nq], in0=ptl[p][:, :nq], in1=mask, op=ALU.mult),
                             reads=["ptl%d" % p, "bm"], writes=["ptl%d" % p])
                    if u.get("accm"):
                        si = u["nacc"]
                        u["nacc"] += 1
                        ub_ = u["uid"] % 2
                        dst_ = accD[ub_][si % 2]
                        dk_ = "accD%d_%d" % (ub_, si % 2)
                        if si == 0:
                            P.op("vector", lambda e, p=p, nq=nq, dst_=dst_: e.tensor_copy(out=dst_[:, :nq], in_=ptl[p][:, :nq]), reads=["ptl%d" % p], writes=[dk_])
                        else:
                            src_ = accD[ub_][(si - 1) % 2]
                            sk_ = "accD%d_%d" % (ub_, (si - 1) % 2)
                            P.op("vector", lambda e, p=p, nq=nq, dst_=dst_, src_=src_: e.tensor_tensor(out=dst_[:, :nq], in0=src_[:, :nq], in1=ptl[p][:, :nq], op=ALU.add),
                                 reads=["ptl%d" % p, sk_], writes=[dk_])
                if i >= LA:
                    u, (kT, v, mask), first, last = steps[i - LA]
                    nq = u["nq"]
                    p = slots.pop(i - LA)
                    ob_ = 4 + (u["uid"] % 2)
                    db_ = 6 + (u["uid"] % 2)
                    P.op("tensor", lambda e, v=v, p=p, ob_=ob_, nq=nq, first=first, last=last: e.matmul(bank[ob_][:, :nq], lhsT=v, rhs=ptl[p][:, :nq], start=first, stop=last),
                         reads=u["rk"] + ["ptl%d" % p], writes=["bank%d" % ob_])
                    if not u.get("accm"):
                        P.op("tensor", lambda e, p=p, db_=db_, nq=nq, first=first, last=last: e.matmul(bank[db_][:, :nq], lhsT=ones[:], rhs=ptl[p][:, :nq], start=first, stop=last),
                             reads=["ones", "ptl%d" % p], writes=["bank%d" % db_])
                    elif last:
                        ui_ = u["uid"] % 2
                        fi_ = (len(u["kblocks"]) - 1) % 2
                        P.op("tensor", lambda e, db_=db_, nq=nq, ui_=ui_, fi_=fi_: e.matmul(bank[db_][:, :nq], lhsT=onesf[:], rhs=accD[ui_][fi_][:, :nq], start=True, stop=True),
                             reads=["onesf", "accD%d_%d" % (ui_, fi_)], writes=["bank%d" % db_])
                    if last:
                        u["epi"](u, ob_, db_)

        def epi_plain(sink_col=None):
            def f(u, ob_, db_):
                nq = u["nq"]
                if sink_col is not None:
                    P.op("vector", lambda e: e.tensor_scalar(out=rec[:, :nq], in0=bank[db_][:, :nq], scalar1=esink[:, sink_col:sink_col + 1], scalar2=None, op0=ALU.add),
                         reads=["bank%d" % db_, "esink"], writes=["rec"])
                    P.op("vector", lambda e: e.reciprocal(out=rec[:, :nq], in_=rec[:, :nq]), reads=["rec"], writes=["rec"])
                else:
                    P.op("vector", lambda e: e.reciprocal(out=rec[:, :nq], in_=bank[db_][:, :nq]), reads=["bank%d" % db_], writes=["rec"])
                P.op("vector", lambda e: e.tensor_tensor(out=u["out"], in0=bank[ob_][:, :nq], in1=rec[:, :nq], op=ALU.mult),
                     reads=["bank%d" % ob_, "rec"], writes=["OT"])
            return f

        def epi_diff(m):
            def f(u, ob_, db_):
                nq = u["nq"]
                dst = oa if m == 0 else obb
                dk = "oa" if m == 0 else "obb"
                P.op("vector", lambda e: e.reciprocal(out=rec[:, :nq], in_=bank[db_][:, :nq]), reads=["bank%d" % db_], writes=["rec"])
                P.op("vector", lambda e: e.tensor_tensor(out=dst[:, :nq], in0=bank[ob_][:, :nq], in1=rec[:, :nq], op=ALU.mult),
                     reads=["bank%d" % ob_, "rec"], writes=[dk])
                if m == 1:
                    P.op("vector", lambda e: e.scalar_tensor_tensor(out=od[:, :nq], in0=obb[:, :nq], scalar=lsc[:, 3:4], in1=oa[:, :nq], op0=ALU.mult, op1=ALU.add),
                         reads=["oa", "obb", "neglam"], writes=["od"])
                    P.op("scalar", lambda e: e.activation(out=osq[:, :nq], in_=od[:, :nq], func=AF.Square), reads=["od"], writes=["osq"])
                    b = state["psi"] % 4
                    state["psi"] += 1
                    P.op("tensor", lambda e: e.matmul(bank[b][:, :nq], lhsT=ones[:], rhs=osq[:, :nq], start=True, stop=True), reads=["ones", "osq"], writes=["bank%d" % b])
                    P.op("scalar", lambda e: e.activation(out=rec[:, :nq], in_=bank[b][:, :nq], func=AF.Sqrt, bias=epst[:], scale=1.0 / 128), reads=["bank%d" % b, "epst"], writes=["rec"])
                    P.op("vector", lambda e: e.reciprocal(out=rec[:, :nq], in_=rec[:, :nq]), reads=["rec"], writes=["rec"])
                    P.op("vector", lambda e: e.scalar_tensor_tensor(out=u["out"], in0=od[:, :nq], scalar=subs[:, 0:1], in1=rec[:, :nq], op0=ALU.mult, op1=ALU.mult),
                         reads=["od", "subs", "rec"], writes=["OT"])
            return f

        qgroups = [(0, 512), (512, 512)] + ([(1024, 64)] if has_ctx else [])

        fence(["KT", "VV", "QQ"])
        KTv = arena[:, 0:2 * KEYS].rearrange("p (h t) -> p h t", h=2)
        VAv = arena[:, 2 * KEYS:2 * KEYS + NKB * 256].rearrange("p (k c) -> p k c", c=256)
        QAv = qar[:, 0:6 * ntq].rearrange("p (h t) -> p h t", h=6)
        for qq in range(4):
            P.op("sync", lambda e, qq=qq: e.dma_start(out=KTv[:, :, qq * TL:(qq + 1) * TL], in_=KTG0[qq * 512:qq * 512 + 256, :].rearrange("(h d) t -> d h t", d=128)), writes=["KT"], dma=True)
            P.op("sync", lambda e, qq=qq: e.dma_start(out=KTv[:, :, SEQ + qq * TC:SEQ + (qq + 1) * TC], in_=KTGc[qq * 1024:qq * 1024 + 256, :].rearrange("(h d) t -> d h t", d=128)), writes=["KT"], dma=True)
        for qq in range(4):
            for c_ in range(2):
                P.op("gpsimd", lambda e, qq=qq, c_=c_: e.dma_start(out=VAv[:, qq * 8 + 4 * c_:qq * 8 + 4 * c_ + 4, :], in_=VGs[c_][qq * 512:(qq + 1) * 512, 0:256].rearrange("(k p) c -> p k c", p=128)), writes=["VV"], dma=True)
        P.op("gpsimd", lambda e: e.dma_start(out=VAv[:, 32:34, :], in_=VGc[:, 0:256].rearrange("(k p) c -> p k c", p=128)), writes=["VV"], dma=True)
        P.op("sync", lambda e: e.dma_start(out=QAv, in_=QT[0:768, 0:ntq].rearrange("(h d) t -> d h t", d=128)), writes=["QQ"], dma=True)
        units = []
        for h in range(6):
            kv = h // 3
            for (q0, nq) in qgroups:
                kbs = range(NKB) if q0 < 1024 else range(32, 34)
                units.append(dict(kblocks=[(KTv[:, kv, kb * 128:(kb + 1) * 128], VAv[:, kb, kv * 128:(kv + 1) * 128], None) for kb in kbs],
                                  q=QAv[:, h, q0:q0 + nq], nq=nq, scale=128 ** -0.5, rk=["KT", "VV", "QQ"], epi=epi_plain(), out=OT[:, h, q0:q0 + nq]))
        run_units(units)

        fence(["KT", "KTc", "VV", "QQ"])
        KBv = arena[:, 0:4 * KEYS].rearrange("p (h t) -> p h t", h=4)
        VBv = arena[:, 4 * KEYS:4 * KEYS + NKB * 512].rearrange("p (k c) -> p k c", c=512)
        QBv = qar[:, 0:4 * ntq].rearrange("p (h t) -> p h t", h=4)
        for m in range(2):
            for qq in range(4):
                P.op("sync", lambda e, m=m, qq=qq: e.dma_start(out=KBv[64 * m:64 * m + 64, :, qq * TL:(qq + 1) * TL], in_=KTG1[qq * 512:qq * 512 + 512, :].rearrange("(h m d) t -> m d h t", m=2, d=64)[m]), writes=["KT"], dma=True)
                P.op("sync", lambda e, m=m, qq=qq: e.dma_start(out=KBv[64 * m:64 * m + 64, :, SEQ + qq * TC:SEQ + (qq + 1) * TC], in_=KTGc[qq * 1024 + 512:qq * 1024 + 1024, :].rearrange("(h m d) t -> m d h t", m=2, d=64)[m]), writes=["KT"], dma=True)
            P.op("sync", lambda e, m=m: e.dma_start(out=QBv[64 * m:64 * m + 64, :, :], in_=QT[768:1280, 0:ntq].rearrange("(h m d) t -> m d h t", m=2, d=64)[m]), writes=["QQ"], dma=True)
        for qq in range(4):
            for c_ in range(2):
                P.op("gpsimd", lambda e, qq=qq, c_=c_: e.dma_start(out=VBv[:, qq * 8 + 4 * c_:qq * 8 + 4 * c_ + 4, :], in_=VGs[c_][qq * 512:(qq + 1) * 512, 256:768].rearrange("(k p) c -> p k c", p=128)), writes=["VV"], dma=True)
        P.op("gpsimd", lambda e: e.dma_start(out=VBv[:, 32:34, :], in_=VGc[:, 256:768].rearrange("(k p) c -> p k c", p=128)), writes=["VV"], dma=True)
        units = []
        for h in range(4):
            for (q0, nq) in qgroups:
                kbs = range(NKB) if q0 < 1024 else range(32, 34)
                for m in range(2):
                    rs = slice(64 * m, 64 * m + 64)
                    units.append(dict(kblocks=[(KBv[rs, h, kb * 128:(kb + 1) * 128], VBv[:, kb, h * 128:(h + 1) * 128], None) for kb in kbs],
                                      q=QBv[rs, h, q0:q0 + nq], nq=nq, scale=64 ** -0.5, rk=["KT", "VV", "QQ"], epi=epi_diff(m), out=OT[:, 6 + h, q0:q0 + nq]))
        run_units(units)

        fence(["KT", "KTc", "VV", "QQ"])
        KCv = arena[:, 0:2 * 1536].rearrange("p (h t) -> p h t", h=2)
        Wt = arena[:, 3072:3072 + 5120].rearrange("p (k c) -> p k c", c=512)
        VCc = arena[:, 8192:8192 + 512].rearrange("p (k c) -> p k c", c=256)
        QCv = qar[:, 0:6 * ntq].rearrange("p (h t) -> p h t", h=6)
        for blk in range(10):
            P.op("gpsimd", lambda e, blk=blk: e.indirect_dma_start(out=Wt[:, blk, :], out_offset=None, in_=WG,
                                                                  in_offset=bass.IndirectOffsetOnAxis(ap=widx[:, blk:blk + 1], axis=0)),
                 reads=["widx"], writes=["VV"], dma=True)
        for qq in range(4):
            P.op("sync", lambda e, qq=qq: e.dma_start(out=KCv[:, :, 1280 + qq * TC:1280 + (qq + 1) * TC], in_=KTGc[qq * 1024 + 256:qq * 1024 + 512, :].rearrange("(h d) t -> d h t", d=128)), writes=["KTc"], dma=True)
        P.op("sync", lambda e: e.dma_start(out=VCc, in_=VGc[:, 768:1024].rearrange("(k p) c -> p k c", p=128)), writes=["VV"], dma=True)
        P.op("sync", lambda e: e.dma_start(out=QCv, in_=QT[1280:2048, 0:ntq].rearrange("(h d) t -> d h t", d=128)), writes=["QQ"], dma=True)
        for rnd in range(5):
            for c in range(4):
                i_ = rnd * 4 + c
                kv, blk = i_ // 10, i_ % 10
                P.op("tensor", lambda e, c=c, kv=kv, blk=blk: e.transpose(out=pTb[:, c, :], in_=Wt[:, blk, 256 + kv * 128:256 + (kv + 1) * 128], identity=idt[:]),
                     reads=["VV", "idt"], writes=["bank7"])
            for c in range(4):
                i_ = rnd * 4 + c
                kv, blk = i_ // 10, i_ % 10
                P.op("scalar", lambda e, c=c, kv=kv, blk=blk: e.copy(out=KCv[:, kv, blk * 128:(blk + 1) * 128], in_=pTb[:, c, :]), reads=["bank7"], writes=["KT"])
        units = []
        for h in range(6):
            kv = h // 3
            ctxk = [(KCv[:, kv, kb * 128:(kb + 1) * 128], VCc[:, kb - 10, kv * 128:(kv + 1) * 128], None) for kb in (10, 11)]
            for n in range(8):
                kbl = []
                for w_ in range(3):
                    kb = n + w_
                    mask = None if w_ == 1 else bm[:, n, 0 if w_ == 0 else 1, :]
                    kbl.append((KCv[:, kv, kb * 128:(kb + 1) * 128], Wt[:, kb, kv * 128:(kv + 1) * 128], mask))
                kbl = kbl + ctxk
                units.append(dict(kblocks=kbl, q=QCv[:, h, n * 128:(n + 1) * 128], nq=128, scale=128 ** -0.5, rk=["KT", "KTc", "VV", "QQ"],
                                  epi=epi_plain(h), out=OT[:, 10 + h, n * 128:(n + 1) * 128]))
            if has_ctx:
                units.append(dict(kblocks=list(ctxk), q=QCv[:, h, 1024:1088], nq=64, scale=128 ** -0.5, rk=["KT", "KTc", "VV", "QQ"],
                                  epi=epi_plain(h), out=OT[:, 10 + h, 1024:1088]))
        run_units(units)

        fence(["wsl0", "wsl1", "wsl0b", "wsl1b", "wsl0c", "wsl1c"] + ["mT%d_%d" % (a_, b_) for a_ in range(4) for b_ in range(ntiles)])
        mT = arena[:, 0:16 * ntq].rearrange("p (k t) -> p k t", k=16)
        wsl = [arena[:, 16 * ntq + s * 8192:16 * ntq + (s + 1) * 8192].rearrange("p (k c) -> p k c", c=512) for s in range(2)]
        wi = 0
        gi = 0

        def load_wbr(i_):
            s_ = i_ % 2
            wk_ = "wsl%d" % s_
            if i_ < 4:
                cs_ = slice(i_ * 512, (i_ + 1) * 512)
                P.op("gpsimd", lambda e: e.dma_start(out=wsl[s_][:, 0:6, :], in_=w_br_a[:, cs_].rearrange("(c p) n -> p c n", p=128)), writes=[wk_, wk_ + "b", wk_ + "c"], dma=True)
                P.op("gpsimd", lambda e: e.dma_start(out=wsl[s_][:, 6:10, :], in_=w_br_b[:, cs_].rearrange("(c p) n -> p c n", p=128)), reads=[wk_], writes=[wk_ + "b"], dma=True)
                P.op("gpsimd", lambda e: e.dma_start(out=wsl[s_][:, 10:16, :], in_=w_br_c[:, cs_].rearrange("(c p) n -> p c n", p=128)), reads=[wk_], writes=[wk_ + "c"], dma=True)
            elif i_ < 8:
                cs_ = slice((i_ - 4) * 512, (i_ - 3) * 512)
                P.op("gpsimd", lambda e: e.dma_start(out=wsl[s_][:], in_=w_out[:, cs_].rearrange("(k p) n -> p k n", p=128)), writes=[wk_, wk_ + "b", wk_ + "c"], dma=True)

        load_wbr(0)
        d1pend = []
        for dg in range(4):
            s = wi % 2
            wi += 1
            wk = "wsl%d" % s
            cs = slice(dg * 512, (dg + 1) * 512)
            load_wbr(wi)
            for t in range(ntiles):
                r = rows(t)
                r0 = t * 128
                g_ = gi % 2
                gi += 1
                gk = "gsl%d" % g_
                P.op("sync", lambda e, g_=g_, r=r, r0=r0, cs=cs: e.dma_start(out=gsl[g_][:r, :, :], in_=Gin[r0:r0 + r, :].rearrange("t (b c) -> t b c", b=3)[:, :, cs]), writes=[gk], dma=True)
                bo = 3 * (g_ % 2)
                for (bi, c0, c1) in ((bo, 0, 6), (bo + 1, 6, 10), (bo + 2, 10, 16)):
                    for c in range(c0, c1):
                        P.op("tensor", lambda e, bi=bi, c=c, c0=c0, c1=c1, s=s, r=r, r0=r0: e.matmul(bank[bi][:r, :], lhsT=OT[:, c, r0:r0 + r], rhs=wsl[s][:, c, :], start=(c == c0), stop=(c == c1 - 1)),
                             reads=["OT", wk, wk + "b", wk + "c"], writes=["bank%d" % bi])
                mk = "mtmp%d" % g_
                P.op("vector", lambda e, g_=g_, r=r, bo=bo: e.tensor_tensor(out=mtmp[g_][:r, :], in0=bank[bo][:r, :], in1=gsl[g_][:r, 0, :], op=ALU.mult), reads=["bank%d" % bo, gk], writes=[mk])
                P.op("vector", lambda e, g_=g_, r=r, bo=bo: e.tensor_tensor(out=xsl[g_][:r, :], in0=bank[bo + 1][:r, :], in1=gsl[g_][:r, 1, :], op=ALU.mult), reads=["bank%d" % (bo + 1), gk], writes=["xsl%d" % g_])
                P.op("vector", lambda e, g_=g_, r=r: e.tensor_tensor(out=mtmp[g_][:r, :], in0=mtmp[g_][:r, :], in1=xsl[g_][:r, :], op=ALU.add), reads=[mk, "xsl%d" % g_], writes=[mk])
                P.op("vector", lambda e, g_=g_, r=r, bo=bo: e.tensor_tensor(out=xsl[g_][:r, :], in0=bank[bo + 2][:r, :], in1=gsl[g_][:r, 2, :], op=ALU.mult), reads=["bank%d" % (bo + 2), gk], writes=["xsl%d" % g_])
                P.op("vector", lambda e, g_=g_, r=r: e.tensor_tensor(out=mb[g_][:r, :], in0=mtmp[g_][:r, :], in1=xsl[g_][:r, :], op=ALU.add), reads=[mk, "xsl%d" % g_], writes=["mb%d" % g_])
                def d1tail(g_=g_, r=r, r0=r0, dg=dg, t=t):
                    for c in range(4):
                        P.op("tensor", lambda e, c=c: e.transpose(out=pTb[:, c, :r], in_=mb[g_][:r, c * 128:(c + 1) * 128], identity=idt[:r, :r]),
                             reads=["mb%d" % g_, "idt"], writes=["bank7"])
                    P.op("scalar", lambda e: e.copy(out=mT[:, dg * 4:(dg + 1) * 4, r0:r0 + r], in_=pTb[:, :, :r]), reads=["bank7"], writes=["mT%d_%d" % (dg, t)])

                d1pend.append(d1tail)
                while len(d1pend) > 1:
                    d1pend.pop(0)()
        for f_ in d1pend:
            f_()

        ui = 0
        for dg in range(4):
            s = wi % 2
            wi += 1
            wk = "wsl%d" % s
            cs = slice(dg * 512, (dg + 1) * 512)
            load_wbr(wi)
            for t in range(ntiles):
                r = rows(t)
                r0 = t * 128
                b = ui % 6
                g_ = ui % 2
                ui += 1
                bk = "bank%d" % b
                P.op("sync", lambda e, g_=g_, r=r, r0=r0, cs=cs: e.dma_start(out=xsl[g_][:r, :], in_=xin[r0:r0 + r, cs]), writes=["xsl%d" % g_], dma=True)
                for k in range(16):
                    P.op("tensor", lambda e, k=k, b=b, s=s, r=r, r0=r0: e.matmul(bank[b][:r, :], lhsT=mT[:, k, r0:r0 + r], rhs=wsl[s][:, k, :], start=(k == 0), stop=(k == 15)),
                         reads=["mT%d_%d" % (k // 4, t), wk], writes=[bk])
                gw = 0 if t < 8 else 1
                P.op("vector", lambda e, b=b, g_=g_, r=r, cs=cs, gw=gw: e.tensor_tensor(out=mtmp[g_][:r, :], in0=bank[b][:r, :], in1=g1t[gw][:r, cs], op=ALU.mult),
                     reads=[bk, "g1t%d" % gw], writes=["mtmp%d" % g_])
                P.op("vector", lambda e, g_=g_, r=r: e.tensor_tensor(out=mtmp[g_][:r, :], in0=mtmp[g_][:r, :], in1=xsl[g_][:r, :], op=ALU.add),
                     reads=["mtmp%d" % g_, "xsl%d" % g_], writes=["mtmp%d" % g_])
                P.op("sync", lambda e, g_=g_, r=r, r0=r0, cs=cs: e.dma_start(out=x1o[r0:r0 + r, cs], in_=mtmp[g_][:r, :]), reads=["mtmp%d" % g_], writes=["x1d%d" % t], dma=True)

        fence(["x1t0", "x1t1", "h2f0", "h2f1", "h2b0", "h2b1", "h2T0", "h2T1", "h2T2", "h2T3", "am2", "bs2"])
        f0_ = abase // 4
        f32v = S.v[F32][:, f0_:f0_ + ARENA // 2]
        x1t = [f32v[:, 0:2048], f32v[:, 12288:14336]]
        h2f = [f32v[:, 2048:4096], f32v[:, 14336:16384]]
        am2 = f32v[:, 4096:6144]
        bs2 = f32v[:, 6144:8192]
        h2T = f32v[:, 8192:10240].rearrange("p (k t) -> p k t", k=16)
        h2b = [arena[:, 20480:22528], arena[:, 22528:24576]]

        def load_mod2(which):
            P.op("sync", lambda e: e.dma_start(out=am2, in_=modv(T, l, which, 4).partition_broadcast(128)), writes=["am2"], dma=True)
            P.op("sync", lambda e: e.dma_start(out=bs2, in_=normf.partition_broadcast(128)), writes=["bs2"], dma=True)
            P.op("vector", lambda e: e.scalar_tensor_tensor(out=am2, in0=am2, scalar=1.0, in1=bs2, op0=ALU.add, op1=ALU.mult), reads=["am2", "bs2"], writes=["am2"])
            P.op("sync", lambda e: e.dma_start(out=bs2, in_=modv(T, l, which, 3).partition_broadcast(128)), reads=["bs2"], writes=["bs2"], dma=True)

        load_mod2(0)

        def stage1(t):
            r = rows(t)
            r0 = t * 128
            u_ = t % 2
            xk_, hk_, bk_ = "x1t%d" % u_, "h2f%d" % u_, "h2b%d" % u_
            if t == 8:
                load_mod2(1)
            P.op("sync", lambda e: e.dma_start(out=x1t[u_][:r, :], in_=x1o[r0:r0 + r, :]), reads=["x1d%d" % t], writes=[xk_], dma=True)
            P.op("scalar", lambda e: e.activation(out=h2f[u_][:r, :], in_=x1t[u_][:r, :], func=AF.Square, accum_out=sm[:r, 0:1]), reads=[xk_], writes=[hk_, "sm0"])
            P.op("scalar", lambda e: e.activation(out=sm[:r, 1:2], in_=sm[:r, 0:1], func=AF.Sqrt, bias=epst[:r, :], scale=1.0 / D), reads=["sm0", "epst"], writes=["sm1"])
            P.op("vector", lambda e: e.reciprocal(out=sm[:r, 1:2], in_=sm[:r, 1:2]), reads=["sm1"], writes=["sm1"])
            P.op("vector", lambda e: e.scalar_tensor_tensor(out=h2f[u_][:r, :], in0=x1t[u_][:r, :], scalar=sm[:r, 1:2], in1=am2[:r, :], op0=ALU.mult, op1=ALU.mult),
                 reads=[xk_, "sm1", "am2"], writes=[hk_])
            P.op("vector", lambda e: e.tensor_tensor(out=h2f[u_][:r, :], in0=h2f[u_][:r, :], in1=bs2[:r, :], op=ALU.add), reads=[hk_, "bs2"], writes=[hk_])
            P.op("scalar", lambda e: e.copy(out=h2b[u_][:r, :], in_=h2f[u_][:r, :]), reads=[hk_], writes=[bk_])
            h2dst = L["h2l%d" % (t // 2)][(t % 2) * 128:(t % 2) * 128 + r, :] if t < 8 else L["h2ctx"][0:r, :]
            P.op("sync", lambda e: e.dma_start(out=h2dst, in_=h2b[u_][:r, :]), reads=[bk_, "cch2_%d" % (t // 2)], dma=True)
            if H2_SPREAD and t < 8 and t % 2 == 1:
                c_ = t // 2
                _cc(P, "AllGather", ALU.bypass, L["h2l%d" % c_], L["h2G"][c_ * 1024:(c_ + 1) * 1024, :], [], ["h2G%d" % c_, "cch2_%d" % c_])

        def stage2(t):
            r = rows(t)
            r0 = t * 128
            u_ = t % 2
            hk_ = "h2f%d" % u_
            for q4 in range(4):
                for c in range(4):
                    k = q4 * 4 + c
                    P.op("tensor", lambda e, q4=q4, c=c, k=k: e.transpose(out=bank[q4][:, c * 128:c * 128 + r], in_=h2f[u_][:r, k * 128:(k + 1) * 128], identity=idf[:r, :r]),
                         reads=[hk_, "idf"], writes=["bank%d" % q4])
                P.op("vector" if q4 % 2 == 0 else "scalar",
                     (lambda e, q4=q4: e.tensor_copy(out=h2T[:, q4 * 4:(q4 + 1) * 4, :r], in_=bank[q4][:].rearrange("p (c t) -> p c t", c=4)[:, :, :r])) if q4 % 2 == 0 else
                     (lambda e, q4=q4: e.copy(out=h2T[:, q4 * 4:(q4 + 1) * 4, :r], in_=bank[q4][:].rearrange("p (c t) -> p c t", c=4)[:, :, :r])),
                     reads=["bank%d" % q4], writes=["h2T%d" % q4])
            for k in range(16):
                P.op("tensor", lambda e, k=k: e.matmul(bank[4][:r, :NEXP], lhsT=h2T[:, k, :r], rhs=wr[:, k, :], start=(k == 0), stop=(k == 15)),
                     reads=["h2T%d" % (k // 4), "wr"], writes=["bank4"])
            a_ = t % 2
            P.op("vector", lambda e: e.tensor_reduce(out=sm[:r, 2:3], in_=bank[4][:r, :NEXP], axis=AX.X, op=ALU.max), reads=["bank4"], writes=["sm2"])
            P.op("vector", lambda e: e.tensor_scalar(out=sm[:r, 2:3], in0=sm[:r, 2:3], scalar1=-1.0, scalar2=None, op0=ALU.mult), reads=["sm2"], writes=["sm2"])
            P.op("scalar", lambda e: e.activation(out=afft[a_][:r, :], in_=bank[4][:r, :NEXP], func=AF.Exp, bias=sm[:r, 2:3], accum_out=sm[:r, 3:4]),
                 reads=["bank4", "sm2"], writes=["afft%d" % a_, "sm3"])
            P.op("vector", lambda e: e.reciprocal(out=sm[:r, 3:4], in_=sm[:r, 3:4]), reads=["sm3"], writes=["sm3"])
            P.op("vector", lambda e: e.tensor_scalar(out=afft[a_][:r, :], in0=afft[a_][:r, :], scalar1=sm[:r, 3:4], scalar2=None, op0=ALU.mult),
                 reads=["afft%d" % a_, "sm3"], writes=["afft%d" % a_])
            P.op("tensor", lambda e: e.transpose(out=bank[5][:NEXP, :r], in_=afft[a_][:r, :], identity=idf[:r, :r]), reads=["afft%d" % a_, "idf"], writes=["bank5"])
            P.op("scalar", lambda e: e.copy(out=affTs[a_][:, :r], in_=bank[5][:NEXP, :r]), reads=["bank5"], writes=["affTs%d" % a_])
            adst = L["affTl"][:, r0:r0 + r] if t < 8 else L["affTc"][:, 0:r]
            P.op("sync", lambda e: e.dma_start(out=adst, in_=affTs[a_][:, :r]), reads=["affTs%d" % a_], dma=True)

        stage1(0)
        for t in range(ntiles):
            if t + 1 < ntiles:
                stage1(t + 1)
            stage2(t)
    S.P.barrier()
    if not H2_SPREAD:
        for c_ in range(4):
            _cc(P, "AllGather", ALU.bypass, L["h2l%d" % c_], L["h2G"][c_ * 1024:(c_ + 1) * 1024, :], [], ["h2G%d" % c_])
    pairs = [("affTl", "affG")] + ([("h2ctx", "h2cG"), ("affTc", "affcG")] if has_ctx else [])
    for (a, b) in pairs:
        _cc(P, "AllGather", ALU.bypass, L[a], L[b], [], [b])


NEL = 4


def phase_C(S, T, l, has_ctx):
    P = S.P
    S.phase()
    L = T["L"][l]
    nt = 5 if has_ctx else 4
    ntok = CAP_L + (CAP_C if has_ctx else 0)
    wg, wu, wd = T["wg"][l], T["wu"][l], T["wd"][l]
    h2G, acc = L["h2G"], L["acc_lat"]
    idx_d, gv_d, alat = L["idx_d"], L["gv_d"], L["alat"]

    def rows(t):
        return 128 if t < 4 else 32

    def row0(t):
        return t * 128

    C = S
    ag = C.sb([16, TL], F32)
    arow = C.sb([16, 1], U32)
    at = C.sb([4, SEQ], F32)
    vals = C.sb([4, CAP_L], F32)
    idx = C.sb([4, CAP_L], U32)
    if has_ctx:
        agc = C.sb([16, TC], F32)
        atc = C.sb([4, CTX], F32)
        valsc = C.sb([4, CAP_C], F32)
        idxc = C.sb([4, CAP_C], U32)
        gTc = C.sb([32, 4], F32)
        idxTc = C.sb([32, 4], U32)
    gT = C.sb([128, 16], F32)
    idxT = C.sb([128, 16], U32)
    zt = C.sb([128, D], F32)
    fsc = C.sb([128, 1], F32)
    idt = C.sb([128, 128], BF16)
    xg = [C.sb([128, D], BF16) for _ in range(2)]
    xgT = C.sb([128, 16, ntok], BF16)
    actT = C.sb([128, 8, ntok], BF16)
    wgs = [C.sb([128, 16, 512], BF16) for _ in range(2)]
    wus = [C.sb([128, 16, 512], BF16) for _ in range(2)]
    wds = C.sb([128, 8, D], BF16)
    su = [C.sb([128, 512], F32) for _ in range(2)]
    yt = [C.sb([128, D], F32) for _ in range(2)]
    pT = C.ps([128, 16, 128], BF16)
    pu = [C.ps([128, 512], F32) for _ in range(2)]
    pv = [C.ps([128, 512], F32) for _ in range(2)]
    py = [C.ps([128, 512], F32) for _ in range(2)]

    P.op("sync", lambda e: e.dma_start(out=idt[:], in_=T["ident"]), writes=["idt"], dma=True)
    P.op("sync", lambda e: e.dma_start(out=arow[:], in_=T["arow"]), writes=["arow"], dma=True)
    PRE = None
    def load_slab(el_, slab_):
        if el_ >= NEL:
            return
        cs_ = slice(slab_ * 512, (slab_ + 1) * 512)
        P.op("gpsimd", lambda e: e.dma_start(out=wgs[slab_][:], in_=wg[el_][:, cs_].rearrange("(k p) f -> p k f", p=128)), writes=["wgs%d" % slab_], dma=True)
        P.op("gpsimd", lambda e: e.dma_start(out=wus[slab_][:], in_=wu[el_][:, cs_].rearrange("(k p) f -> p k f", p=128)), writes=["wus%d" % slab_], dma=True)

    def load_wd(el_):
        if el_ >= NEL:
            return
        P.op("gpsimd", lambda e: e.dma_start(out=wds[:], in_=wd[el_].rearrange("(f p) d -> p f d", p=128)), writes=["wds"], dma=True)

    load_wd(0)
    load_slab(0, 0)
    load_slab(0, 1)
    P.op("gpsimd", lambda e: e.indirect_dma_start(out=ag[:, :], out_offset=None, in_=L["affG"], in_offset=bass.IndirectOffsetOnAxis(ap=arow[:, 0:1], axis=0)),
         reads=["arow"], writes=["ag"], dma=True)
    P.op("sync", lambda e: e.dma_start(out=alat, in_=ag[:]), reads=["ag"], writes=["alat"], dma=True)
    for c_ in range(4):
        P.op("sync", lambda e, c_=c_: e.dma_start(out=at[:, c_ * 1024:(c_ + 1) * 1024].rearrange("e (q t) -> e q t", q=4), in_=alat.rearrange("(e q) (c t) -> e c q t", q=4, c=4)[:, c_]),
             reads=["alat"], writes=["at%d" % c_], dma=True)
    if has_ctx:
        P.op("gpsimd", lambda e: e.indirect_dma_start(out=agc[:, :], out_offset=None, in_=L["affcG"], in_offset=bass.IndirectOffsetOnAxis(ap=arow[:, 0:1], axis=0)),
             reads=["arow"], writes=["agc"], dma=True)
        P.op("sync", lambda e: e.dma_start(out=L["actx"], in_=agc[:]), reads=["agc"], writes=["actx"], dma=True)
        P.op("sync", lambda e: e.dma_start(out=atc[:], in_=L["actx"].rearrange("(e q) t -> e (q t)", q=4)), reads=["actx"], writes=["atc"], dma=True)
    P.op("gpsimd", lambda e: e.memset(zt[:], 0.0), writes=["zt"])
    zkeys = []
    zi = 0
    for b0 in range(0, SEQ // 128, 4):
        zk = "z%d" % zi
        zkeys.append(zk)
        P.op("sync" if zi % 2 == 0 else "gpsimd",
             lambda e, b0=b0: e.dma_start(out=acc[b0 * 128:(b0 + 4) * 128, :].rearrange("(n p) d -> p n d", p=128), in_=zt[:].unsqueeze(1).to_broadcast([128, 4, D])),
             reads=["zt"], writes=[zk], dma=True)
        zi += 1
    if has_ctx:
        zkeys.append("zc")
        P.op("sync", lambda e: e.dma_start(out=L["acc_ctx"].rearrange("(n p) d -> p n d", p=128), in_=zt[:].unsqueeze(1).to_broadcast([128, 2, D])), reads=["zt"], writes=["zc"], dma=True)
    gkeys = ["accg0", "accg1"]
    P.op("gpsimd", lambda e: e.memset(fsc[:], 0.0), reads=zkeys, writes=gkeys + ["fsc"])

    def topk(src, v_, i_, k, nm):
        for it in range(k // 8):
            sl = slice(it * 8, (it + 1) * 8)
            P.op("vector", lambda e, sl=sl: e.max(out=v_[:, sl], in_=src[:]), reads=[nm], writes=[nm + "v"])
            P.op("vector", lambda e, sl=sl: e.max_index(out=i_[:, sl], in_max=v_[:, sl], in_values=src[:]), reads=[nm, nm + "v"], writes=[nm + "i"])
            if it < k // 8 - 1:
                P.op("vector", lambda e, sl=sl: e.match_replace(out=src[:], in_to_replace=v_[:, sl], in_values=src[:], imm_value=-1.0), reads=[nm + "v", nm], writes=[nm])

    P.op("vector", lambda e: e.memset(fsc[0:4, :], 0.0), reads=["at0", "at1", "at2", "at3"], writes=["at", "fsc"])
    topk(at, vals, idx, CAP_L, "at")
    if has_ctx:
        topk(atc, valsc, idxc, CAP_C, "atc")
    P.op("sync", lambda e: e.dma_start(out=idx_d, in_=idx[:]), reads=["ati"], writes=["idx_d"], dma=True)
    P.op("sync", lambda e: e.dma_start(out=gv_d, in_=vals[:]), reads=["atv"], writes=["gv_d"], dma=True)
    P.op("sync", lambda e: e.dma_start(out=idxT[:].rearrange("p (r j) -> p r j", r=4), in_=idx_d.rearrange("r (j p) -> p r j", p=128), allow_slow_non_contiguous=True),
         reads=["idx_d"], writes=["idxT"], dma=True)
    P.op("sync", lambda e: e.dma_start(out=gT[:].rearrange("p (r j) -> p r j", r=4), in_=gv_d.rearrange("r (j p) -> p r j", p=128), allow_slow_non_contiguous=True),
         reads=["gv_d"], writes=["gT"], dma=True)
    if has_ctx:
        P.op("sync", lambda e: e.dma_start(out=L["idxc_d"], in_=idxc[:]), reads=["atci"], writes=["idxc_d"], dma=True)
        P.op("sync", lambda e: e.dma_start(out=L["gvc_d"], in_=valsc[:]), reads=["atcv"], writes=["gvc_d"], dma=True)
        P.op("sync", lambda e: e.dma_start(out=idxTc[:], in_=L["idxc_d"].rearrange("r c -> c r"), allow_slow_non_contiguous=True),
             reads=["idxc_d"], writes=["idxT"], dma=True)
        P.op("sync", lambda e: e.dma_start(out=gTc[:], in_=L["gvc_d"].rearrange("r c -> c r"), allow_slow_non_contiguous=True),
             reads=["gvc_d"], writes=["gT"], dma=True)

    cgroups = [(0, 512)] + ([(512, 32)] if has_ctx else [])

    xi = 0
    wi = 0
    ui = 0
    yi = 0
    for el in range(NEL):
        for t in range(nt):
            r = rows(t)
            r0 = row0(t)
            x_ = xi % 2
            xi += 1
            xk = "xg%d" % x_
            if t < 4:
                col = el * 4 + t
                P.op("gpsimd", lambda e, x_=x_, col=col: e.indirect_dma_start(out=xg[x_][:, :], out_offset=None, in_=h2G,
                                                                             in_offset=bass.IndirectOffsetOnAxis(ap=idxT[:, col:col + 1], axis=0)),
                     reads=["idxT"], writes=[xk], dma=True)
            else:
                P.op("gpsimd", lambda e, x_=x_, el=el: e.indirect_dma_start(out=xg[x_][0:32, :], out_offset=None, in_=L["h2cG"],
                                                                           in_offset=bass.IndirectOffsetOnAxis(ap=idxTc[:, el:el + 1], axis=0)),
                     reads=["idxT"], writes=[xk], dma=True)
            for k in range(16):
                P.op("tensor", lambda e, k=k, x_=x_, r=r: e.transpose(out=pT[:, k, :r], in_=xg[x_][:r, k * 128:(k + 1) * 128], identity=idt[:r, :r]),
                     reads=[xk, "idt"], writes=["pT"])
            P.op("scalar", lambda e, r=r, r0=r0: e.copy(out=xgT[:, :, r0:r0 + r], in_=pT[:, :, :r]), reads=["pT"], writes=["xgT%d" % t])
        for slab in range(2):
            w_ = slab
            cs = slice(slab * 512, (slab + 1) * 512)
            for fbl in range(4):
                fb = slab * 4 + fbl
                fs = slice(fbl * 128, (fbl + 1) * 128)
                for (c0, n) in cgroups:
                    u_ = ui % 2
                    ui += 1
                    tk = ["xgT%d" % t for t in (range(4) if c0 == 0 else (4,))]
                    for k in range(16):
                        P.op("tensor", lambda e, k=k, u_=u_, w_=w_, fs=fs, c0=c0, n=n: e.matmul(pu[u_][:, :n], lhsT=wgs[w_][:, k, fs], rhs=xgT[:, k, c0:c0 + n], start=(k == 0), stop=(k == 15)),
                             reads=tk + ["wgs%d" % w_], writes=["pu%d" % u_])
                    for k in range(16):
                        P.op("tensor", lambda e, k=k, u_=u_, w_=w_, fs=fs, c0=c0, n=n: e.matmul(pv[u_][:, :n], lhsT=wus[w_][:, k, fs], rhs=xgT[:, k, c0:c0 + n], start=(k == 0), stop=(k == 15)),
                             reads=tk + ["wus%d" % w_], writes=["pv%d" % u_])
                    P.op("scalar", lambda e, u_=u_, n=n: e.activation(out=su[u_][:, :n], in_=pu[u_][:, :n], func=AF.Silu), reads=["pu%d" % u_], writes=["su%d" % u_])
                    P.op("vector", lambda e, u_=u_, fb=fb, c0=c0, n=n: e.tensor_tensor(out=actT[:, fb, c0:c0 + n], in0=su[u_][:, :n], in1=pv[u_][:, :n], op=ALU.mult),
                         reads=["su%d" % u_, "pv%d" % u_], writes=["actT%d_%d" % (fb, c0)])
                    if fbl == 3 and (c0, n) == cgroups[-1]:
                        load_slab(el + 1, slab)
        for t in range(nt):
            r = rows(t)
            r0 = row0(t)
            y_ = yi % 2
            yi += 1
            yk = "yt%d" % y_
            cg0 = 0 if t < 4 else 512
            ak = ["actT%d_%d" % (f, cg0) for f in range(8)]
            if t < 4:
                col = el * 4 + t
                gsc = gT[:, col:col + 1]
            else:
                gsc = gTc[:, el:el + 1]
            for dg in range(4):
                b_ = (t * 4 + dg) % 2
                for f in range(8):
                    P.op("tensor", lambda e, f=f, b_=b_, dg=dg, r=r, r0=r0: e.matmul(py[b_][:r, :], lhsT=actT[:, f, r0:r0 + r], rhs=wds[:, f, dg * 512:(dg + 1) * 512], start=(f == 0), stop=(f == 7)),
                         reads=ak + ["wds"], writes=["py%d" % b_])
                P.op("vector", lambda e, b_=b_, dg=dg, r=r, y_=y_, gsc=gsc: e.tensor_scalar(out=yt[y_][:r, dg * 512:(dg + 1) * 512], in0=py[b_][:r, :], scalar1=gsc[:r, :], scalar2=None, op0=ALU.mult),
                     reads=["py%d" % b_, "gT"], writes=[yk + "_%d" % dg])
            ykeys = [yk + "_%d" % dg for dg in range(4)]
            if t < 4:
                P.op("gpsimd", lambda e, y_=y_, col=col: e.indirect_dma_start(out=acc, out_offset=bass.IndirectOffsetOnAxis(ap=idxT[:, col:col + 1], axis=0),
                                                                             in_=yt[y_][:, :], in_offset=None, compute_op=ALU.add),
                     reads=ykeys + ["idxT"], writes=["accg0"], dma=True)
            else:
                P.op("gpsimd", lambda e, y_=y_, el=el: e.indirect_dma_start(out=L["acc_ctx"], out_offset=bass.IndirectOffsetOnAxis(ap=idxTc[:, el:el + 1], axis=0),
                                                                           in_=yt[y_][0:32, :], in_offset=None, compute_op=ALU.add),
                     reads=ykeys + ["idxT"], writes=["accg1"], dma=True)
            if t == nt - 1:
                load_wd(el + 1)
    S.P.barrier()
    for c_ in range(4):
        _cc(P, "ReduceScatter", ALU.add, L["acc_lat"][c_ * 1024:(c_ + 1) * 1024, :], L["comb%d" % c_], [], ["comb%d" % c_])
    if has_ctx:
        _cc(P, "ReduceScatter", ALU.add, L["acc_ctx"], L["comb_ctx"], [], ["comb_ctx"])


def build_fused():
    nc = bass.Bass("TRN2", target_bir_lowering=False)
    T = {}
    ein = lambda name, shape, dt: T.__setitem__(name, _din(nc, name, shape, dt))
    ein("xin", [TT, D], F32)
    ein("cT", [128, 16, 2], F32)
    ein("wm", [D, MW], F32)
    ein("bm", [MW], F32)
    ein("norm_mix", [2, D], F32)
    ein("norm_ffn", [2, D], F32)
    ein("w_in", [2, D, IN_W], F32)
    ein("g128", [2, 4, 128], F32)
    ein("g64", [2, 2, 64], F32)
    ein("cos128", [TL, 128], F32)
    ein("sin128", [TL, 128], F32)
    ein("cos64", [TL, 64], F32)
    ein("sin64", [TL, 64], F32)
    ein("ident", [128, 128], BF16)
    ein("identf", [128, 128], F32)
    ein("bandm", [128, 8, 2, 128], BF16)
    ein("widx", [128, 10], U32)
    ein("arow", [16, 1], U32)
    ein("w_br_a", [2, 768, D], F32)
    ein("w_br_b", [2, 512, D], F32)
    ein("w_br_c", [2, 768, D], F32)
    ein("w_out", [2, D, D], F32)
    ein("w_router", [2, D, NEXP], F32)
    ein("lamv", [2, 4, 64], F32)
    ein("subln", [2, 128], F32)
    ein("sink", [2, 6], F32)
    ein("wg", [2, NEL, D, FF], F32)
    ein("wu", [2, NEL, D, FF], F32)
    ein("wd", [2, NEL, FF, D], F32)
    out = _dout(nc, "out", [TL, D], F32)
    with contextlib.ExitStack() as es:
        S = State(nc, es)
        T["modloc"] = S.dram([2, MW], F32)
        T["modG"] = S.dram([8, MW], F32)
        T["L"] = []
        for l in range(2):
            L = {}
            for (nm, shape, dt) in (("KTloc0", [512, TL], BF16), ("KTG0", [2048, TL], BF16), ("KTloc1", [512, TL], BF16), ("KTG1", [2048, TL], BF16),
                                    ("KTlocC", [1024, TC], BF16), ("KTGc", [4096, TC], BF16),
                                    ("Vloc0", [512, 1024], BF16), ("VG0", [2048, 1024], BF16), ("Vloc1", [512, 1024], BF16), ("VG1", [2048, 1024], BF16),
                                    ("VlocC", [TC, 1024], BF16), ("VGc", [CTX, 1024], BF16),
                                    ("WKVloc", [TL, 512], BF16), ("WG", [SEQ, 512], BF16), ("QT", [2048, TT], BF16), ("G", [TT, 6144], BF16),
                                    ("x1", [TT, D], F32), ("xcomb", [TT, D], F32),
                                    ("h2l0", [256, D], BF16), ("h2l1", [256, D], BF16), ("h2l2", [256, D], BF16), ("h2l3", [256, D], BF16), ("h2G", [SEQ, D], BF16), ("h2ctx", [TC, D], BF16), ("h2cG", [CTX, D], BF16),
                                    ("affTl", [NEXP, TL], F32), ("affG", [4 * NEXP, TL], F32), ("affTc", [NEXP, TC], F32), ("affcG", [4 * NEXP, TC], F32),
                                    ("alat", [16, TL], F32), ("actx", [16, TC], F32),
                                    ("idx_d", [4, CAP_L], U32), ("gv_d", [4, CAP_L], F32), ("idxc_d", [4, CAP_C], U32), ("gvc_d", [4, CAP_C], F32),
                                    ("acc_lat", [SEQ, D], F32), ("comb0", [256, D], F32), ("comb1", [256, D], F32), ("comb2", [256, D], F32), ("comb3", [256, D], F32), ("acc_ctx", [CTX, D], F32), ("comb_ctx", [TC, D], F32)):
                L[nm] = S.dram(shape, dt, "%s_%d" % (nm, l))
            T["L"].append(L)
        phase_M(S, T)
        phase_A(S, T, 0, False, True, 9, T["xin"], None)
        phase_B(S, T, 0, 9, T["xin"])
        phase_C(S, T, 0, True)
        phase_A(S, T, 1, True, True, 9, T["L"][0]["x1"], T["L"][1]["xcomb"])
        phase_B(S, T, 1, 8, T["L"][1]["xcomb"])
        phase_C(S, T, 1, False)
        phase_A(S, T, 2, True, False, 8, T["L"][1]["x1"], out, final_out=True)
        S.P.emit()
    return nc


_IDENT_B = np.eye(128).astype(NPBF)
_IDENT_F = np.eye(128, dtype=np.float32)
_NC = {}


def _rope_tables(dh):
    d_ax = dh // 2
    inv = (np.float32(10000.0) ** (-np.arange(0, d_ax, 2, dtype=np.float32) / np.float32(d_ax))).astype(np.float32)
    t = np.arange(SEQ)
    row = (t // 64).astype(np.float32)[:, None]
    col = (t % 64).astype(np.float32)[:, None]
    fr = row * inv
    fc = col * inv
    ang = np.concatenate([fr, fr, fc, fc], axis=-1).astype(np.float32)
    cos = np.cos(ang).astype(np.float32)
    sin = np.sin(ang).astype(np.float32)
    q = dh // 4
    sgn = np.concatenate([-np.ones(q), np.ones(q), -np.ones(q), np.ones(q)]).astype(np.float32)
    return cos, (sin * sgn[None, :]).astype(np.float32)


def _band_masks(j):
    kp = np.arange(128)[:, None]
    qf = np.arange(128)[None, :]
    m = np.zeros((128, 8, 2, 128), np.float32)
    for n in range(8):
        gn = 8 * j + n
        if gn - 1 >= 0:
            m[:, n, 0, :] = (qf <= kp)
        if gn + 1 <= 31:
            m[:, n, 1, :] = (kp <= qf)
    return m.astype(NPBF)


def kernel(**inp):
    inp = {k: np.ascontiguousarray(np.asarray(v)) for k, v in inp.items()}
    if "nc" not in _NC:
        _NC["nc"] = build_fused()
    nc = _NC["nc"]
    cos128, sin128 = _rope_tables(128)
    cos64, sin64 = _rope_tables(64)
    wm_all = np.concatenate([inp["w_mod"][0], inp["w_mod"][1]], axis=1)
    bm_all = np.concatenate([inp["b_mod"][0], inp["b_mod"][1]], axis=0)
    shared = dict(
        norm_mix=inp["norm_mix"], norm_ffn=inp["norm_ffn"], w_in=inp["w_in"],
        g128=np.ascontiguousarray(np.stack([inp["qn_a"], inp["kn_a"], inp["qn_c"], inp["kn_c"]], axis=1)),
        g64=np.ascontiguousarray(np.stack([inp["qn_b"], inp["kn_b"]], axis=1)),
        ident=_IDENT_B, identf=_IDENT_F,
        w_br_a=inp["w_br_a"], w_br_b=inp["w_br_b"], w_br_c=inp["w_br_c"], w_out=inp["w_out"], w_router=inp["w_router"],
        lamv=np.ascontiguousarray(np.stack([inp["lam_q1"], inp["lam_k1"], inp["lam_q2"], inp["lam_k2"]], axis=1)),
        subln=inp["subln_b"], sink=inp["sink_c"])
    maps = []
    for i in range(NCORE):
        s, q = i // 4, i % 4
        cst = np.stack([inp["c"][s], inp["c_ctx"]], axis=0)
        cT = np.ascontiguousarray(cst.T.reshape(16, 128, 2).transpose(1, 0, 2))
        sl = slice(q * TL, (q + 1) * TL)
        widx = np.zeros((128, 10), np.uint32)
        for blk in range(10):
            gb = min(max(8 * q - 1 + blk, 0), 31)
            widx[:, blk] = gb * 128 + np.arange(128)
        arow = np.array([[qq * NEXP + 4 * q + el] for el in range(NEL) for qq in range(4)], np.uint32)
        m = dict(shared)
        m.update(
            xin=np.ascontiguousarray(np.concatenate([inp["x"][s, sl], inp["ctx"][s, q * TC:(q + 1) * TC]], 0)),
            cT=cT, wm=np.ascontiguousarray(wm_all[:, q * MW:(q + 1) * MW]), bm=np.ascontiguousarray(bm_all[q * MW:(q + 1) * MW]),
            cos128=cos128[sl], sin128=sin128[sl], cos64=cos64[sl], sin64=sin64[sl],
            bandm=_band_masks(q), widx=widx, arow=arow,
            wg=np.ascontiguousarray(inp["w_gate"][:, 4 * q:4 * q + 4]), wu=np.ascontiguousarray(inp["w_up"][:, 4 * q:4 * q + 4]),
            wd=np.ascontiguousarray(inp["w_down"][:, 4 * q:4 * q + 4]))
        maps.append(m)
    res = run_bass_kernel_spmd(nc, maps, core_ids=list(range(NCORE)))
    outs = [np.asarray(r["out"]) for r in res.results]
    out = np.stack([np.concatenate(outs[4 * s:4 * s + 4], 0) for s in range(2)])
    return out.astype(np.float32)
```
